# Optimizing a Trainium2 kernel written in Bass

```python
import math
import jax
import jax.numpy as jnp
from jax import lax
import numpy as np

D_MODEL = 1024
BATCH = 8
SEQ = 2048
DEPTH = 4

CTX_LEN = 256
GRID_W = 64

HEAD_DIM = 64
N_Q_HEADS = 8
N_KV_HEADS = 2
Q_PER_KV = N_Q_HEADS // N_KV_HEADS
ATTN_WIDTH = N_Q_HEADS * HEAD_DIM
KV_WIDTH = N_KV_HEADS * HEAD_DIM
POOL_WINDOWS = (2, 4, 8, 16)
N_POOL_GROUPS = len(POOL_WINDOWS)
POOL_WIDTH = D_MODEL - ATTN_WIDTH
POOL_GROUP_DIM = POOL_WIDTH // N_POOL_GROUPS
IN_WIDTH = ATTN_WIDTH + 2 * KV_WIDTH + POOL_WIDTH
MIX_WIDTH = ATTN_WIDTH + POOL_WIDTH
WINDOW = 128
Q_BLOCK = 128
ROPE_BASE = 10000.0
ROPE_FREQS = HEAD_DIM // 4

D_FF_DENSE = 2816
N_EXPERTS = 8
TOP_K = 2
D_FF_EXPERT = 3584
N_DENSE = (DEPTH + 1) // 2
N_MOE = DEPTH // 2

ALPHA = (2 * DEPTH) ** 0.25
BETA = (8 * DEPTH) ** -0.25
LN_EPS = 1e-6
NEG_INF = -1e30

kernel_name = "hybrid_pool_window_gqa_moe_dit"


def _layernorm(x):
    xf = x.astype(jnp.float32)
    mu = jnp.mean(xf, axis=-1, keepdims=True)
    var = jnp.mean(jnp.square(xf - mu), axis=-1, keepdims=True)
    return ((xf - mu) * lax.rsqrt(var + LN_EPS)).astype(x.dtype)


def _post_norm(residual, update, gate, g, b):
    return _layernorm(ALPHA * residual + gate[:, None, :] * update) * g + b


def _modulate(x, shift, scale):
    return _layernorm(x) * (1.0 + scale[:, None, :]) + shift[:, None, :]


def _axial_rope_tables(n_tokens):
    rows = n_tokens // GRID_W
    row = jnp.repeat(jnp.arange(rows, dtype=jnp.float32), GRID_W)
    col = jnp.tile(jnp.arange(GRID_W, dtype=jnp.float32), rows)
    inv = ROPE_BASE ** (-jnp.arange(ROPE_FREQS, dtype=jnp.float32) / ROPE_FREQS)
    ang_r = row[:, None] * inv[None, :]
    ang_c = col[:, None] * inv[None, :]
    return (jnp.cos(ang_r), jnp.sin(ang_r), jnp.cos(ang_c), jnp.sin(ang_c))


def _rotate_half(x, cos, sin):
    x1, x2 = jnp.split(x, 2, axis=-1)
    cos = cos[None, :, None, :]
    sin = sin[None, :, None, :]
    return jnp.concatenate([x1 * cos - x2 * sin, x2 * cos + x1 * sin], axis=-1)


def _apply_axial_rope(x, tables):
    cos_r, sin_r, cos_c, sin_c = tables
    xr, xc = jnp.split(x.astype(jnp.float32), 2, axis=-1)
    out = jnp.concatenate([_rotate_half(xr, cos_r, sin_r), _rotate_half(xc, cos_c, sin_c)], axis=-1)
    return out.astype(x.dtype)


def _split_proj(p):
    b, n = p.shape[0], p.shape[1]
    q = p[..., :ATTN_WIDTH].reshape(b, n, N_Q_HEADS, HEAD_DIM)
    k = p[..., ATTN_WIDTH:ATTN_WIDTH + KV_WIDTH].reshape(b, n, N_KV_HEADS, HEAD_DIM)
    v = p[..., ATTN_WIDTH + KV_WIDTH:ATTN_WIDTH + 2 * KV_WIDTH].reshape(b, n, N_KV_HEADS, HEAD_DIM)
    u = p[..., ATTN_WIDTH + 2 * KV_WIDTH:]
    return q, k, v, u


def _latent_window_attention(q, k, v, k_ctx, v_ctx, sink):
    b, n_lat = q.shape[0], q.shape[1]
    nb = n_lat // Q_BLOCK
    n_ctx = k_ctx.shape[1]
    scale = HEAD_DIM ** -0.5
    qb = (q * scale).reshape(b, nb, Q_BLOCK, N_KV_HEADS, Q_PER_KV, HEAD_DIM)
    pad = ((0, 0), (WINDOW, WINDOW), (0, 0), (0, 0))
    kp = jnp.pad(k, pad).reshape(b, nb + 2, Q_BLOCK, N_KV_HEADS, HEAD_DIM)
    vp = jnp.pad(v, pad).reshape(b, nb + 2, Q_BLOCK, N_KV_HEADS, HEAD_DIM)
    kb = jnp.concatenate([kp[:, :-2], kp[:, 1:-1], kp[:, 2:]], axis=2)
    vb = jnp.concatenate([vp[:, :-2], vp[:, 1:-1], vp[:, 2:]], axis=2)
    s_band = jnp.einsum('bnqkgd,bnjkd->bnkgqj', qb, kb, preferred_element_type=jnp.float32)
    blk = jnp.arange(nb)[:, None, None]
    qi = jnp.arange(Q_BLOCK)[None, :, None]
    kj = jnp.arange(3 * Q_BLOCK)[None, None, :]
    pos_k = blk * Q_BLOCK - WINDOW + kj
    valid = (jnp.abs(kj - WINDOW - qi) <= WINDOW) & (pos_k >= 0) & (pos_k < n_lat)
    s_band = jnp.where(valid[None, :, None, None, :, :], s_band, NEG_INF)
    s_ctx = jnp.einsum('bnqkgd,bckd->bnkgqc', qb, k_ctx, preferred_element_type=jnp.float32)
    s_sink = jnp.broadcast_to(sink.astype(jnp.float32).reshape(1, 1, N_KV_HEADS, Q_PER_KV, 1, 1),
                              s_band.shape[:-1] + (1,))
    p = jax.nn.softmax(jnp.concatenate([s_band, s_ctx, s_sink], axis=-1), axis=-1)
    n_band = 3 * Q_BLOCK
    p_band = p[..., :n_band].astype(v.dtype)
    p_ctx = p[..., n_band:n_band + n_ctx].astype(v.dtype)
    out = (jnp.einsum('bnkgqj,bnjkd->bnqkgd', p_band, vb)
           + jnp.einsum('bnkgqc,bckd->bnqkgd', p_ctx, v_ctx))
    return out.reshape(b, n_lat, ATTN_WIDTH)


def _context_attention(q, k, v, sink):
    b, n_ctx = q.shape[0], q.shape[1]
    qg = (q * HEAD_DIM ** -0.5).reshape(b, n_ctx, N_KV_HEADS, Q_PER_KV, HEAD_DIM)
    s = jnp.einsum('bqkgd,bckd->bkgqc', qg, k, preferred_element_type=jnp.float32)
    s_sink = jnp.broadcast_to(sink.astype(jnp.float32).reshape(1, N_KV_HEADS, Q_PER_KV, 1, 1),
                              s.shape[:-1] + (1,))
    p = jax.nn.softmax(jnp.concatenate([s, s_sink], axis=-1), axis=-1)[..., :n_ctx].astype(v.dtype)
    out = jnp.einsum('bkgqc,bckd->bqkgd', p, v)
    return out.reshape(b, n_ctx, ATTN_WIDTH)


def _multiscale_pool(u, w_pool, pool_scale):
    b, n = u.shape[0], u.shape[1]
    ug = u.reshape(b, n, N_POOL_GROUPS, POOL_GROUP_DIM)
    cs = jnp.cumsum(ug.astype(jnp.float32), axis=1)
    cs = jnp.concatenate([jnp.zeros_like(cs[:, :1]), cs], axis=1)
    t = jnp.arange(n)
    pooled = []
    for g, w in enumerate(POOL_WINDOWS):
        lo = jnp.clip(t - w // 2, 0, n)
        hi = jnp.clip(t + w // 2, 0, n)
        cnt = (hi - lo).astype(jnp.float32)[None, :, None]
        pooled.append((cs[:, hi, g] - cs[:, lo, g]) / cnt)
    pooled = jnp.stack(pooled, axis=2).astype(u.dtype)
    mixed = jnp.einsum('bngd,gde->bnge', pooled - ug, w_pool)
    return mixed.reshape(b, n, POOL_WIDTH) * pool_scale


def _token_mixer(a_lat, a_ctx, w_in, w_pool, pool_scale, sink, w_out, rope, need_ctx_out):
    q, k, v, u = _split_proj(a_lat @ w_in)
    qc, kc, vc, uc = _split_proj(a_ctx @ w_in)
    q = _apply_axial_rope(q, rope)
    k = _apply_axial_rope(k, rope)
    attn = _latent_window_attention(q, k, v, kc, vc, sink)
    pool = _multiscale_pool(u, w_pool, pool_scale)
    y_lat = jnp.concatenate([attn, pool], axis=-1) @ w_out
    if not need_ctx_out:
        return y_lat, None
    attn_c = _context_attention(qc, kc, vc, sink)
    pool_c = _multiscale_pool(uc, w_pool, pool_scale)
    y_ctx = jnp.concatenate([attn_c, pool_c], axis=-1) @ w_out
    return y_lat, y_ctx


def _swiglu(h, w1, w3, w2):
    return (jax.nn.silu(h @ w1) * (h @ w3)) @ w2


def _moe_swiglu(h, router, w1, w3, w2):
    logits = jnp.einsum('bnd,de->bne', h, router, preferred_element_type=jnp.float32)
    top_vals, top_idx = lax.top_k(logits, TOP_K)
    top_w = jax.nn.softmax(top_vals, axis=-1)
    gates = jnp.sum(jax.nn.one_hot(top_idx, N_EXPERTS, dtype=jnp.float32) * top_w[..., None], axis=-2)
    gates = gates.astype(h.dtype)
    out = jnp.zeros_like(h)
    for e in range(N_EXPERTS):
        out = out + gates[..., e:e + 1] * _swiglu(h, w1[e], w3[e], w2[e])
    return out


def setup_inputs(seed: int = 0) -> dict:
    key = jax.random.key(seed)
    ks = jax.random.split(key, 24)
    nrm = jax.random.normal
    f32 = jnp.float32
    d = D_MODEL
    return {
        "x": nrm(ks[0], (BATCH, SEQ, d), f32),
        "c": nrm(ks[1], (BATCH, d), f32),
        "ctx": nrm(ks[2], (BATCH, CTX_LEN, d), f32),
        "c_ctx": nrm(ks[3], (d,), f32),
        "w_ada": nrm(ks[4], (DEPTH, d, 6 * d), f32) * (0.5 * d ** -0.5),
        "b_ada": nrm(ks[5], (DEPTH, 6 * d), f32) * 0.02,
        "w_in": nrm(ks[6], (DEPTH, d, IN_WIDTH), f32) * d ** -0.5,
        "w_pool": nrm(ks[7], (DEPTH, N_POOL_GROUPS, POOL_GROUP_DIM, POOL_GROUP_DIM), f32) * POOL_GROUP_DIM ** -0.5,
        "pool_scale": 1.0 + 0.02 * nrm(ks[8], (DEPTH, POOL_WIDTH), f32),
        "sink": 0.5 * nrm(ks[9], (DEPTH, N_Q_HEADS), f32),
        "w_out": nrm(ks[10], (DEPTH, MIX_WIDTH, d), f32) * (BETA * MIX_WIDTH ** -0.5),
        "ln_g": 1.0 + 0.02 * nrm(ks[11], (DEPTH, 2, d), f32),
        "ln_b": 0.02 * nrm(ks[12], (DEPTH, 2, d), f32),
        "dense_w1": nrm(ks[13], (N_DENSE, d, D_FF_DENSE), f32) * d ** -0.5,
        "dense_w3": nrm(ks[14], (N_DENSE, d, D_FF_DENSE), f32) * d ** -0.5,
        "dense_w2": nrm(ks[15], (N_DENSE, D_FF_DENSE, d), f32) * (BETA * D_FF_DENSE ** -0.5),
        "router": nrm(ks[16], (N_MOE, d, N_EXPERTS), f32) * d ** -0.5,
        "moe_w1": nrm(ks[17], (N_MOE, N_EXPERTS, d, D_FF_EXPERT), f32) * d ** -0.5,
        "moe_w3": nrm(ks[18], (N_MOE, N_EXPERTS, d, D_FF_EXPERT), f32) * d ** -0.5,
        "moe_w2": nrm(ks[19], (N_MOE, N_EXPERTS, D_FF_EXPERT, d), f32) * (BETA * D_FF_EXPERT ** -0.5),
    }


def reference(x, c, ctx, c_ctx, w_ada, b_ada, w_in, w_pool, pool_scale, sink, w_out, ln_g, ln_b,
              dense_w1, dense_w3, dense_w2, router, moe_w1, moe_w3, moe_w2):
    rope = _axial_rope_tables(x.shape[1])
    silu_c = jax.nn.silu(c)
    silu_cc = jax.nn.silu(c_ctx)[None, :]
    h, hc = x, ctx
    for l in range(DEPTH):
        need_ctx = l < DEPTH - 1
        mod = silu_c @ w_ada[l] + b_ada[l]
        mod_c = silu_cc @ w_ada[l] + b_ada[l]
        sh1, sc1, g1, sh2, sc2, g2 = jnp.split(mod, 6, axis=-1)
        csh1, csc1, cg1, csh2, csc2, cg2 = jnp.split(mod_c, 6, axis=-1)

        y_lat, y_ctx = _token_mixer(_modulate(h, sh1, sc1), _modulate(hc, csh1, csc1),
                                    w_in[l], w_pool[l], pool_scale[l], sink[l], w_out[l], rope, need_ctx)
        h = _post_norm(h, y_lat, g1, ln_g[l, 0], ln_b[l, 0])
        if need_ctx:
            hc = _post_norm(hc, y_ctx, cg1, ln_g[l, 0], ln_b[l, 0])

        a = _modulate(h, sh2, sc2)
        if l % 2 == 0:
            i = l // 2
            f_lat = _swiglu(a, dense_w1[i], dense_w3[i], dense_w2[i])
        else:
            i = l // 2
            f_lat = _moe_swiglu(a, router[i], moe_w1[i], moe_w3[i], moe_w2[i])
        h = _post_norm(h, f_lat, g2, ln_g[l, 1], ln_b[l, 1])
        if need_ctx:
            ac = _modulate(hc, csh2, csc2)
            if l % 2 == 0:
                f_ctx = _swiglu(ac, dense_w1[i], dense_w3[i], dense_w2[i])
            else:
                f_ctx = _moe_swiglu(ac, router[i], moe_w1[i], moe_w3[i], moe_w2[i])
            hc = _post_norm(hc, f_ctx, cg2, ln_g[l, 1], ln_b[l, 1])
    return h
```

```python
import numpy as np
import ml_dtypes
from contextlib import ExitStack
import concourse.bass as bass
import concourse.mybir as mybir
from concourse.bass_utils import run_bass_kernel_spmd

F32 = mybir.dt.float32
BF16 = mybir.dt.bfloat16
AF = mybir.ActivationFunctionType
ALU = mybir.AluOpType
AX = mybir.AxisListType

D = 1024
KC = 8
NLAT = 2048
NCTX = 256
T = NLAT + NCTX
DEPTH = 4
TGS = [(0, 512), (512, 512), (1024, 512), (1536, 512), (2048, 256)]
NTILE = T // 128
POOL_WINDOWS = (2, 4, 8, 16)
DFF_DENSE = 2816
DFF_MOE = 3584
NEXP = 8
ALPHA = (2 * DEPTH) ** 0.25
LN_EPS = 1e-6
LP = 2336
ULAT = 8
UCTX = 2072
MASKNEG = -240000.0
SLOT = 6144
NSLOT = 3
WIN_EXT = 2176


class Tok:
    __slots__ = ("key", "sem", "val", "clock")

    def __init__(self, key, sem, val, clock):
        self.key, self.sem, self.val, self.clock = key, sem, val, clock


class Reg:
    __slots__ = ("name", "w", "r", "dsem", "dcnt")

    def __init__(self, name):
        self.name, self.w, self.r, self.dsem, self.dcnt = name, None, {}, None, 0


class Eng:
    def __init__(self, name, h, sem):
        self.name, self.h, self.sem, self.cnt, self.seen, self.last = name, h, sem, 0, {}, None
        self.pending = []


class Sched:
    def __init__(self, nc, es):
        self.nc, self.es = nc, es
        self.eng = {}
        for name, h in (("pe", nc.tensor), ("act", nc.scalar), ("dve", nc.vector),
                        ("pool", nc.gpsimd), ("sp", nc.sync)):
            self.eng[name] = Eng(name, h, es.enter_context(nc.semaphore("sem_" + name)))
        self.nsem = 0

    def _deps(self, E, R, W):
        deps = list(E.pending)
        E.pending = []
        for r in R:
            if r.w is not None:
                deps.append(r.w)
        for w in W:
            if w.w is not None and (w.w.key != E.name or E.name != "pe"):
                deps.append(w.w)
            for k, t in w.r.items():
                if k != E.name or E.name != "pe":
                    deps.append(t)
        return deps

    def _wait(self, E, deps):
        for t in deps:
            if E.seen.get(t.key, 0) >= t.val:
                continue
            E.h.wait_ge(t.sem, t.val)
            E.seen[t.key] = t.val
            for k, v in t.clock.items():
                if E.seen.get(k, 0) < v:
                    E.seen[k] = v

    def op(self, en, fns, R=(), W=()):
        E = self.eng[en]
        self._wait(E, self._deps(E, R, W))
        if callable(fns):
            fns = [fns]
        ins = None
        for f in fns:
            ins = f(E.h)
        E.cnt += 1
        ins.then_inc(E.sem, 1)
        clock = dict(E.seen)
        clock[E.name] = E.cnt
        t = Tok(E.name, E.sem, E.cnt, clock)
        E.last = t
        for r in R:
            r.r[E.name] = t
        for w in W:
            w.w = t
            w.r = {}
        return t

    def dma(self, qn, out, in_, R=(), W=(), sem_reg=None):
        E = self.eng[qn]
        self._wait(E, self._deps(E, R, W))
        reg = sem_reg if sem_reg is not None else W[0]
        if reg.dsem is None:
            reg.dsem = self.es.enter_context(self.nc.semaphore("dsem%d" % self.nsem))
            self.nsem += 1
        reg.dcnt += 16
        E.h.dma_start(out=out, in_=in_).then_inc(reg.dsem, 16)
        t = Tok(("d", id(reg)), reg.dsem, reg.dcnt, dict(E.seen))
        for r in R:
            r.r[t.key] = t
        for w in W:
            w.w = t
            w.r = {}
        return t

    def barrier(self, names=("pe", "act", "dve", "sp")):
        toks = [self.eng[n].last for n in names if self.eng[n].last is not None]
        for n in names:
            self.eng[n].pending = [t for t in toks if t.key != n]


class Buf:
    __slots__ = ("ap", "reg")

    def __init__(self, ap, name):
        self.ap, self.reg = ap, Reg(name)


class Ring:
    def __init__(self, bufs):
        self.bufs, self.i = bufs, 0

    def next(self):
        b = self.bufs[self.i % len(self.bufs)]
        self.i += 1
        return b


def MM(out, lhsT, rhs, st=True, sp=True):
    return lambda h: h.matmul(out, lhsT, rhs, start=st, stop=sp)


def TR(out, in_, ident):
    return lambda h: h.transpose(out, in_, ident)


def ACTF(out, in_, func, **kw):
    return lambda h: h.activation(out=out, in_=in_, func=func, **kw)


def TT(out, a, b, op):
    return lambda h: h.tensor_tensor(out=out, in0=a, in1=b, op=op)


def TS(out, a, s1, s2, op0, op1=None):
    if op1 is None:
        return lambda h: h.tensor_scalar(out=out, in0=a, scalar1=s1, scalar2=None, op0=op0)
    return lambda h: h.tensor_scalar(out=out, in0=a, scalar1=s1, scalar2=s2, op0=op0, op1=op1)


def STT(out, a, s, b, op0, op1):
    return lambda h: h.scalar_tensor_tensor(out=out, in0=a, scalar=s, in1=b, op0=op0, op1=op1)


def RED(out, in_, op, axis=None):
    return lambda h: h.tensor_reduce(out=out, in_=in_, axis=(axis or AX.X), op=op)


def RECIP(out, in_):
    return lambda h: h.reciprocal(out=out, in_=in_)


def COPY(out, in_):
    return lambda h: h.tensor_copy(out=out, in_=in_)


def MEMSET(ap, v):
    return lambda h: h.memset(ap, v)


def build(layers=(0, 1, 2, 3), dbg=None):
    nc = bass.Bass("TRN2", target_bir_lowering=False)
    es = ExitStack()

    def din(name, shape, dt=F32):
        return nc.dram_tensor(name, list(shape), dt, kind="ExternalInput").ap()

    xT = din("xT", [D, NLAT])
    ctxT = din("ctxT", [D, NCTX])
    cc_d = din("cc", [128, KC, 2])
    bada_d = din("badaT", [128, DEPTH, 48])
    lngb_d = din("lngb", [128, DEPTH * 2 * 2 * KC])
    psc_d = din("pscale", [128, DEPTH * 4])
    sink_d = din("sinkb", [128, DEPTH * 8])
    rout_d = din("routerT", [128, 2 * KC * NEXP])
    tabs_d = din("tabs", [128, 4, 2, 512])
    mask_d = din("mask", [128, 384], BF16)
    idb_d = din("identb", [128, 128], BF16)
    idf_d = din("identf", [128, 128])
    onesf_d = din("onesf", [128, 128])
    edge_d = din("edge", [128, 4 * 2 * 8])
    eps_d = din("epsc", [128, 2])
    onesd_d = din("onesd", [128, 128])
    w_ada = din("w_ada", [DEPTH, D, 6 * D])
    w_inx = din("w_inx", [DEPTH, D, WIN_EXT])
    w_pool = din("w_pool", [DEPTH, 4, 128, 128])
    w_out = din("w_out", [DEPTH, D, D])
    dw1 = din("dense_w1", [2, D, DFF_DENSE])
    dw3 = din("dense_w3", [2, D, DFF_DENSE])
    dw2 = din("dense_w2", [2, DFF_DENSE, D])
    mw1 = din("moe_w1", [2, NEXP, D, DFF_MOE])
    mw3 = din("moe_w3", [2, NEXP, D, DFF_MOE])
    mw2 = din("moe_w2", [2, NEXP, DFF_MOE, D])
    outT = nc.dram_tensor("outT", [D, NLAT], F32, kind="ExternalOutput").ap()
    if dbg is not None:
        dbg_d = nc.dram_tensor("dbg", [D, T], F32, kind="ExternalOutput").ap()

    S = Sched(nc, es)

    uniq = [0]

    def sb(name, shape, dt=F32, st=es):
        uniq[0] += 1
        return st.enter_context(nc.sbuf_tensor("%s_%d" % (name, uniq[0]), list(shape), dt))

    def ps(name, shape, dt=F32):
        return es.enter_context(nc.psum_tensor(name, list(shape), dt))

    H = sb("H", [128, KC, T])
    A = sb("A", [128, KC, T], BF16)
    Hr = [[Reg("H%d_%d" % (k, g)) for g in range(5)] for k in range(KC)]
    Ar = [[Reg("A%d_%d" % (k, g)) for g in range(5)] for k in range(KC)]
    RINGT = sb("ring", [128, NSLOT, SLOT], BF16)
    slots = [[Buf(RINGT[:, s, j * 2048:(j + 1) * 2048], "slot%d_%d" % (s, j)) for j in range(3)]
             for s in range(NSLOT)]
    slot_i = [0]
    WP = sb("wp", [128, 1, 4, 128], BF16)
    WPb = [Buf(WP[:, 0], "wp0")] * 2
    TMPT = sb("tmp", [128, 5, 644])
    TMP = Ring([Buf(TMPT[:, i], "tmp%d" % i) for i in range(5)])
    STT_ = sb("stat", [128, 3, 512])
    STAT = [Buf(STT_[:, i], "stat%d" % i) for i in range(3)]
    SMT = sb("small", [128, 8, 4])
    SM = Ring([Buf(SMT[:, i], "sm%d" % i) for i in range(8)])
    CC = sb("ccs", [128, KC, 2])
    CCB = sb("ccb", [128, KC, 2], BF16)
    BADA = sb("bada", [128, DEPTH, 48])
    MOD = sb("mod", [128, DEPTH, 48, 2])
    LNGB = sb("lngbs", [128, DEPTH * 2 * 2 * KC])
    PSC = sb("pscs", [128, DEPTH * 4])
    SK = sb("sk", [128, DEPTH * 8])
    ROUT = sb("routs", [128, 2 * KC * NEXP])
    MASK = sb("masks", [128, 384], BF16)
    IDB = sb("idb", [128, 128], BF16)
    IDF = sb("idf", [128, 128])
    ONESF = sb("onesfs", [128, 128])
    EDGE = sb("edges", [128, 4 * 2 * 8])
    EPSC = sb("epscs", [128, 2])
    ONESD = sb("onesds", [128, 128])
    LG = sb("lg", [128, NTILE, NEXP])
    GATES = sb("gates", [128, NTILE, NEXP])
    Vt = sb("V", [128, NTILE, 128], BF16)
    constR = Reg("const")
    modRs = [Reg("mod%d" % i) for i in range(DEPTH)]
    ccR = Reg("cc")
    lgR = Reg("lg")
    gatesR = Reg("gates")
    gtR = Reg("gt")
    Vr = [Reg("V%d" % i) for i in range(NTILE)]

    S0 = ps("S0", [128, 1024])
    S1 = ps("S1", [128, 1024])
    B = [ps("B%d" % i, [128, 512]) for i in range(4)]
    SC = [Buf(S0, "S0"), Buf(S1, "S1")]
    MMB = Ring([Buf(S0[:, 0:512], "mm0"), Buf(S0[:, 512:1024], "mm1"),
                Buf(S1[:, 0:512], "mm2"), Buf(S1[:, 512:1024], "mm3")])
    MMB.bufs[0].reg = MMB.bufs[1].reg = SC[0].reg
    MMB.bufs[2].reg = MMB.bufs[3].reg = SC[1].reg
    MMB.bufs = [MMB.bufs[0], MMB.bufs[2], MMB.bufs[1], MMB.bufs[3]]
    BB = [Buf(B[i], "B%d" % i) for i in range(4)]
    BR = Ring(BB)

    def mod_ap(l, which, k, s):
        return MOD[:, l, which * 8 + k, s:s + 1]

    def tgs_of(g):
        return 0 if g < 4 else 1

    for dst, src in ((CC, cc_d), (BADA, bada_d), (LNGB, lngb_d), (PSC, psc_d), (SK, sink_d),
                     (ROUT, rout_d), (MASK, mask_d), (IDB, idb_d), (IDF, idf_d),
                     (ONESF, onesf_d), (EDGE, edge_d), (EPSC, eps_d), (ONESD, onesd_d)):
        S.dma("sp", dst[:], src, W=[constR])
    for k in range(KC):
        S.dma("sp", H[:, k, 0:NLAT], xT[k * 128:(k + 1) * 128, :], W=Hr[k][0:4])
        S.dma("sp", H[:, k, NLAT:T], ctxT[k * 128:(k + 1) * 128, :], W=[Hr[k][4]])
    S.op("act", ACTF(CCB[:], CC[:], AF.Silu), R=[constR], W=[ccR])
    S.op("dve", TS(LNGB[:, 0:112], LNGB[:, 0:112], float(ALPHA), None, ALU.mult), R=[constR], W=[constR])

    def next_slot():
        s = slots[slot_i[0] % NSLOT]
        slot_i[0] += 1
        return s

    def load_slot(src_ap, ncols, kdim=KC):
        s = next_slot()
        base = s[0].ap.tensor if False else None
        si = (slot_i[0] - 1) % NSLOT
        view = RINGT[:, si, 0:kdim * ncols].rearrange("p (k n) -> p k n", k=kdim)
        S.dma("pool", view, src_ap.rearrange("(k p) n -> p k n", p=128),
              W=[s[0].reg, s[1].reg, s[2].reg])
        return view, [s[0].reg, s[1].reg, s[2].reg]

    def mods_steps(l):
        pm = BB[0]
        steps = []

        def piece(sidx):
            view, regs = load_slot(w_ada[l, :, sidx * 768:(sidx + 1) * 768], 768)
            for jj in range(6):
                j = sidx * 6 + jj
                S.op("pe", [MM(pm.ap[:, 2 * j:2 * j + 2], view[:, k, jj * 128:(jj + 1) * 128],
                               CCB[:, k, :], k == 0, k == KC - 1) for k in range(KC)],
                     R=regs + [ccR], W=[pm.reg])

        def finish():
            for s_ in range(2):
                S.op("dve", TT(MOD[:, l, :, s_], pm.ap[:, 0:96].rearrange("p (j s) -> p j s", s=2)[:, :, s_],
                               BADA[:, l, :], ALU.add), R=[pm.reg, constR], W=[modRs[l]])
            for which in (1, 4):
                S.op("dve", TS(MOD[:, l, which * 8:(which + 1) * 8, :], MOD[:, l, which * 8:(which + 1) * 8, :],
                               1.0, None, ALU.add), R=[modRs[l]], W=[modRs[l]])

        for sidx in range(8):
            steps.append(lambda sidx=sidx: piece(sidx))
        steps.append(finish)
        return steps

    for l_ in list(layers)[:2]:
        for st_ in mods_steps(l_):
            st_()

    stat_i = [0]

    def ln_A(g):
        t0, n = TGS[g]
        pa, pb = (BB[0], BB[1]) if stat_i[0] % 2 == 0 else (BB[2], BB[3])
        stat_i[0] += 1
        for k in range(KC):
            sq = TMP.next()
            S.op("act", ACTF(sq.ap[:, :n], H[:, k, t0:t0 + n], AF.Square), R=[Hr[k][g]], W=[sq.reg])
            S.op("pe", [MM(pa.ap[:, :n], ONESD[:], H[:, k, t0:t0 + n], k == 0, k == KC - 1),
                        MM(pb.ap[:, :n], ONESD[:], sq.ap[:, :n], k == 0, k == KC - 1)],
                 R=[Hr[k][g], sq.reg, constR], W=[pa.reg, pb.reg])
        return pa, pb

    def ln_chain(g, pa, pb, epsi):
        t0, n = TGS[g]
        msq, rstd, nmr = STAT
        S.op("act", ACTF(msq.ap[:, :n], pa.ap[:, :n], AF.Square), R=[pa.reg], W=[msq.reg])
        S.op("dve", TT(rstd.ap[:, :n], pb.ap[:, :n], msq.ap[:, :n], ALU.subtract), R=[pb.reg, msq.reg], W=[rstd.reg])
        S.op("act", ACTF(rstd.ap[:, :n], rstd.ap[:, :n], AF.Sqrt, bias=EPSC[:, epsi:epsi + 1]),
             R=[rstd.reg, constR], W=[rstd.reg])
        S.op("dve", RECIP(rstd.ap[:, :n], rstd.ap[:, :n]), R=[rstd.reg], W=[rstd.reg])
        S.op("dve", STT(nmr.ap[:, :n], pa.ap[:, :n], -1.0, rstd.ap[:, :n], ALU.mult, ALU.mult),
             R=[pa.reg, rstd.reg], W=[nmr.reg])

    def ln_apply(g, emit):
        t0, n = TGS[g]
        msq, rstd, nmr = STAT
        for k0 in range(0, KC, 2):
            xs = [TMP.next(), TMP.next()]
            for j in range(2):
                S.op("dve", TT(xs[j].ap[:, :n], H[:, k0 + j, t0:t0 + n], rstd.ap[:, :n], ALU.mult),
                     R=[Hr[k0 + j][g], rstd.reg], W=[xs[j].reg])
            for j in range(2):
                S.op("dve", TT(xs[j].ap[:, :n], xs[j].ap[:, :n], nmr.ap[:, :n], ALU.add),
                     R=[xs[j].reg, nmr.reg], W=[xs[j].reg])
            for j in range(2):
                emit(k0 + j, xs[j], n)

    def run_ln_jobs(jobs):
        stats = ln_A(jobs[0][0])
        for ji, (g, emit, epsi, post) in enumerate(jobs):
            pa, pb = stats
            ln_chain(g, pa, pb, epsi)
            if ji + 1 < len(jobs):
                stats = ln_A(jobs[ji + 1][0])
            ln_apply(g, emit)
            if post is not None:
                post()

    RL = Buf(S0[:, 0:512], "rl")
    RL.reg = SC[0].reg

    def modulate_jobs(l, sub, router_i=None, groups=range(5), first=False):
        wsh, wsc = (0, 1) if sub == 0 else (3, 4)
        jobs = []
        for g in groups:
            t0, n = TGS[g]
            s_ = tgs_of(g)
            rl = RL

            def emit(k, xh, n, g=g, t0=t0, s_=s_, rl=rl):
                if router_i is None:
                    S.op("act", ACTF(A[:, k, t0:t0 + n], xh.ap[:, :n], AF.Identity,
                                     scale=mod_ap(l, wsc, k, s_), bias=mod_ap(l, wsh, k, s_)),
                         R=[xh.reg, modRs[l]], W=[Ar[k][g]])
                else:
                    S.op("act", ACTF(xh.ap[:, :n], xh.ap[:, :n], AF.Identity,
                                     scale=mod_ap(l, wsc, k, s_), bias=mod_ap(l, wsh, k, s_)),
                         R=[xh.reg, modRs[l]], W=[xh.reg])
                    S.op("act", ACTF(A[:, k, t0:t0 + n], xh.ap[:, :n], AF.Copy), R=[xh.reg], W=[Ar[k][g]])
                    S.op("pe", MM(rl.ap[0:NEXP, :n],
                                  ROUT[:, (router_i * KC + k) * NEXP:(router_i * KC + k + 1) * NEXP],
                                  xh.ap[:, :n], k == 0, k == KC - 1), R=[xh.reg, constR], W=[rl.reg])
                if first:
                    S.op("act", ACTF(H[:, k, t0:t0 + n], H[:, k, t0:t0 + n], AF.Copy, scale=float(ALPHA)),
                         R=[Hr[k][g]], W=[Hr[k][g]])

            def post(t0=t0, n=n, rl=rl):
                lt = TMP.next()
                S.op("act", ACTF(lt.ap[0:NEXP, :n], rl.ap[0:NEXP, :n], AF.Copy), R=[rl.reg], W=[lt.reg])
                nt = n // 128
                S.op("pe", [MM(rl.ap[:, i * NEXP:(i + 1) * NEXP],
                               lt.ap[0:NEXP, i * 128:(i + 1) * 128], IDF[0:NEXP, 0:NEXP], True, True)
                            for i in range(nt)], R=[lt.reg, constR], W=[rl.reg])
                S.op("dve", COPY(LG[:, t0 // 128:t0 // 128 + nt, :],
                                 rl.ap[:, 0:nt * NEXP].rearrange("p (i e) -> p i e", e=NEXP)),
                     R=[rl.reg], W=[lgR])

            jobs.append((g, emit, (0 if first else 1), post if router_i is not None else None))
        return jobs

    def postnorm_jobs(l, sub, groups=range(5)):
        jobs = []
        for g in groups:
            t0, n = TGS[g]

            def emit(k, xh, n, g=g, t0=t0):
                gi = ((l * 2 + sub) * 2 + 0) * KC + k
                bi = ((l * 2 + sub) * 2 + 1) * KC + k
                S.op("act", ACTF(H[:, k, t0:t0 + n], xh.ap[:, :n], AF.Identity,
                                 scale=LNGB[:, gi:gi + 1], bias=LNGB[:, bi:bi + 1]),
                     R=[xh.reg, constR], W=[Hr[k][g]])

            jobs.append((g, emit, 0, None))
        return jobs

    def accum_H(l, which, m, g, pbuf, n):
        t0 = TGS[g][0]
        S.op("dve", STT(H[:, m, t0:t0 + n], pbuf.ap[:, :n], mod_ap(l, which, m, tgs_of(g)),
                        H[:, m, t0:t0 + n], ALU.mult, ALU.add),
             R=[pbuf.reg, modRs[l], Hr[m][g]], W=[Hr[m][g]])

    def token_mixer(l):
        last = (l == DEPTH - 1)
        GO = range(4) if last else range(5)
        Uv, Ur = load_slot(w_inx[l, :, 0:640], 640)
        wpb = WPb[l % 2]
        S.dma("pool", wpb.ap, w_pool[l].rearrange("g d e -> d g e"), W=[wpb.reg])
        Yv, Yr = load_slot(w_out[l, 512:1024, :], 1024, kdim=4)
        with ExitStack() as ph:
            Up = Buf(sb("Up", [128, LP], F32, ph), "Up")
            Sw = Buf(sb("Sw", [128, LP], F32, ph), "Sw")
            Dd = Buf(sb("Dd", [128, T], BF16, ph), "Dd")
            PMt = sb("PM", [128, 2, T], BF16, ph)
            PMr = [[Reg("pm%d_%d" % (i, g)) for g in range(5)] for i in range(2)]
            LPE = (ULAT + NLAT + 8) if last else LP
            seqs = ((ULAT, 0, NLAT),) if last else ((ULAT, 0, NLAT), (UCTX, NLAT, NCTX))
            pads = ((0, ULAT), (ULAT + NLAT, LPE)) if last else ((0, ULAT), (ULAT + NLAT, UCTX), (UCTX + NCTX, LP))
            for gi, w in enumerate(POOL_WINDOWS):
                hw = w // 2
                for lo, hi in pads:
                    S.op("dve", MEMSET(Up.ap[:, lo:hi], 0.0), W=[Up.reg])
                for g in GO:
                    t0, n = TGS[g]
                    pb = MMB.next()
                    S.op("pe", [MM(pb.ap[:, :n], Uv[:, k, gi * 128:(gi + 1) * 128], A[:, k, t0:t0 + n],
                                   k == 0, k == KC - 1) for k in range(KC)],
                         R=Ur + [Ar[k][g] for k in range(KC)], W=[pb.reg])
                    u0 = ULAT + t0 if g < 4 else UCTX
                    S.op("act", ACTF(Up.ap[:, u0:u0 + n], pb.ap[:, :n], AF.Copy), R=[pb.reg], W=[Up.reg])
                S.op("dve", TT(Sw.ap[:, 0:LPE - 1], Up.ap[:, 0:LPE - 1], Up.ap[:, 1:LPE], ALU.add),
                     R=[Up.reg], W=[Sw.reg])
                sh, ext = 2, LPE - 1
                while sh < w:
                    ext -= sh
                    S.op("dve", TT(Sw.ap[:, 0:ext], Sw.ap[:, 0:ext], Sw.ap[:, sh:sh + ext], ALU.add),
                         R=[Sw.reg], W=[Sw.reg])
                    sh *= 2
                for bu, bd, n in seqs:
                    S.op("dve", STT(Dd.ap[:, bd:bd + n], Sw.ap[:, bu - hw:bu - hw + n], 1.0 / w,
                                    Up.ap[:, bu:bu + n], ALU.mult, ALU.subtract),
                         R=[Sw.reg, Up.reg], W=[Dd.reg])
                    e0 = (gi * 2 + 0) * 8
                    tl = TMP.next()
                    S.op("dve", TT(tl.ap[:, 0:hw], Sw.ap[:, bu - hw:bu], EDGE[:, e0:e0 + hw], ALU.mult),
                         R=[Sw.reg, constR], W=[tl.reg])
                    S.op("dve", TT(Dd.ap[:, bd:bd + hw], tl.ap[:, 0:hw], Up.ap[:, bu:bu + hw], ALU.subtract),
                         R=[tl.reg, Up.reg], W=[Dd.reg])
                    if hw > 1:
                        e1 = (gi * 2 + 1) * 8
                        c0 = n - hw + 1
                        tr_ = TMP.next()
                        S.op("dve", TT(tr_.ap[:, 0:hw - 1], Sw.ap[:, bu + c0 - hw:bu + n - hw],
                                       EDGE[:, e1:e1 + hw - 1], ALU.mult), R=[Sw.reg, constR], W=[tr_.reg])
                        S.op("dve", TT(Dd.ap[:, bd + c0:bd + n], tr_.ap[:, 0:hw - 1], Up.ap[:, bu + c0:bu + n],
                                       ALU.subtract), R=[tr_.reg, Up.reg], W=[Dd.reg])
                for g in GO:
                    t0, n = TGS[g]
                    pb = MMB.next()
                    S.op("pe", MM(pb.ap[:, :n], wpb.ap[:, gi, :], Dd.ap[:, t0:t0 + n]),
                         R=[wpb.reg, Dd.reg], W=[pb.reg])
                    S.op("act", ACTF(PMt[:, gi % 2, t0:t0 + n], pb.ap[:, :n], AF.Identity,
                                     scale=PSC[:, l * 4 + gi:l * 4 + gi + 1]),
                         R=[pb.reg, constR], W=[PMr[gi % 2][g]])
                if gi % 2 == 1:
                    for m in range(KC):
                        for g in GO:
                            t0, n = TGS[g]
                            pb = MMB.next()
                            S.op("pe", [MM(pb.ap[:, :n], Yv[:, gi - 1 + kk, m * 128:(m + 1) * 128],
                                           PMt[:, kk, t0:t0 + n], kk == 0, kk == 1) for kk in range(2)],
                                 R=Yr + [PMr[0][g], PMr[1][g]], W=[pb.reg])
                            accum_H(l, 2, m, g, pb, n)
            for i in range(NTILE):
                g = min(i // 4, 4)
                pb = MMB.next()
                S.op("pe", [MM(pb.ap[:, 0:128], A[:, k, i * 128:(i + 1) * 128], Uv[:, k, 512:640],
                               k == 0, k == KC - 1) for k in range(KC)],
                     R=Ur + [Ar[k][g] for k in range(KC)], W=[pb.reg])
                S.op("act", ACTF(Vt[:, i, :], pb.ap[:, 0:128], AF.Copy), R=[pb.reg], W=[Vr[i]])
            S.barrier()
        with ExitStack() as ph:
            Qt = sb("Q", [128, 2, T], BF16, ph)
            Kt = sb("K", [128, T], BF16, ph)
            Ot = sb("O", [128, 2, T], BF16, ph)
            TABt = sb("TAB", [128, 2, 512], F32, ph)
            Qr = [[Reg("q%d_%d" % (c, g)) for g in range(5)] for c in range(2)]
            Kr = [Reg("k%d" % g) for g in range(5)]
            Or = [[Reg("o%d_%d" % (c, g)) for g in range(5)] for c in range(2)]
            tabR = Reg("tab")
            TF = TMPT[:].rearrange("p a b -> p (a b)")
            PF = [Buf(TF[:, i * 644:(i + 1) * 644], "pf%d" % i) for i in range(2)]
            TB = TF[:, 1288:3220].bitcast(BF16)
            PN = [Buf(TB[:, i * 640:(i + 1) * 640], "pn%d" % i) for i in range(3)]
            PTs = [Buf(TB[:, (3 + i) * 640:(4 + i) * 640], "pt%d" % i) for i in range(3)]
            PTP = [Buf(BB[i].ap.bitcast(BF16), "ptp%d" % i) for i in range(2)]
            PTP[0].reg, PTP[1].reg = BB[0].reg, BB[1].reg
            OB = [BB[2], BB[3]]
            Gs = [load_slot(w_inx[l, :, 640:640 + 768], 768)]
            Xv, Xr = load_slot(w_out[l, 0:512, :], 1024, kdim=4)
            Gs.append(load_slot(w_inx[l, :, 640 + 768:640 + 2 * 768], 768))
            nqb = 16 if last else NTILE
            for kvg in range(2):
                Gv, Gr = Gs[kvg]
                for g in range(5):
                    t0, n = TGS[g]
                    if g < 4:
                        S.dma("sp", TABt[:], tabs_d[:, g], W=[tabR])
                    for c in range(3):
                        if c < 2 and g == 4 and last:
                            continue
                        ca, cb = (c, c + 2) if c < 2 else (4, 5)
                        dst = Qt[:, c, t0:t0 + n] if c < 2 else Kt[:, t0:t0 + n]
                        dreg = Qr[c][g] if c < 2 else Kr[g]
                        qs = 0.125 if c < 2 else 1.0
                        pa = MMB.next()
                        S.op("pe", [MM(pa.ap[:, :n], Gv[:, k, ca * 128:(ca + 1) * 128], A[:, k, t0:t0 + n],
                                       k == 0, k == KC - 1) for k in range(KC)],
                             R=Gr + [Ar[k][g] for k in range(KC)], W=[pa.reg])
                        if g == 4:
                            S.op("act", ACTF(dst, pa.ap[:, :n], AF.Copy, scale=qs), R=[pa.reg], W=[dreg])
                            continue
                        pb = MMB.next()
                        S.op("pe", [MM(pb.ap[:, :n], Gv[:, k, cb * 128:(cb + 1) * 128], A[:, k, t0:t0 + n],
                                       k == 0, k == KC - 1) for k in range(KC)],
                             R=Gr + [Ar[k][g] for k in range(KC)], W=[pb.reg])
                        t1, t2 = TMP.next(), TMP.next()
                        S.op("dve", TT(t1.ap[:, :n], pa.ap[:, :n], TABt[:, 0, :n], ALU.mult),
                             R=[pa.reg, tabR], W=[t1.reg])
                        S.op("dve", STT(t2.ap[:, :n], pb.ap[:, :n], qs, TABt[:, 1, :n], ALU.mult, ALU.mult),
                             R=[pb.reg, tabR], W=[t2.reg])
                        S.op("dve", STT(dst, t1.ap[:, :n], qs, t2.ap[:, :n], ALU.mult, ALU.add),
                             R=[t1.reg, t2.reg], W=[dreg])
                S.barrier()
                pairs = [(lc, qb, hh) for lc in range(2) for qb in range(nqb) for hh in range(2)]
                st = {}

                def stage1(pi):
                    lc, qb, hh = pairs[pi]
                    head = kvg * 4 + lc * 2 + hh
                    p0 = hh * 64
                    sc = SC[pi % 2]
                    gq = min(qb // 4, 4)
                    Qh = Qt[p0:p0 + 64, lc, qb * 128:(qb + 1) * 128]
                    if qb >= 16:
                        kb0, kb1, nb = 0, -1, 0
                    else:
                        kb0, kb1 = max(qb - 1, 0), min(qb + 1, 15)
                        nb = (kb1 - kb0 + 1) * 128
                    lo = 512 - nb
                    nk = nb + 256
                    fns = []
                    if nb:
                        fns.append(MM(sc.ap[:, lo:512], Qh, Kt[p0:p0 + 64, kb0 * 128:(kb1 + 1) * 128], True, False))
                    fns.append(MM(sc.ap[:, 512:768], Qh, Kt[p0:p0 + 64, NLAT:T], True, True))
                    if nb:
                        mm_ = []
                        if kb0 == qb - 1:
                            mm_.append((lo, 0))
                        if kb1 == qb + 1:
                            mm_.append((512 - 128, 256))
                        for ii, (c_, mo) in enumerate(mm_):
                            fns.append(MM(sc.ap[:, c_:c_ + 128], IDB[:], MASK[:, mo:mo + 128], False, ii == len(mm_) - 1))
                    kregs = [Kr[4]] + ([Kr[kb // 4] for kb in range(kb0, kb1 + 1)] if nb else [])
                    S.op("pe", fns, R=[Qr[lc][gq], constR] + kregs, W=[sc.reg])
                    sm = SM.next()
                    skh = SK[:, l * 8 + head:l * 8 + head + 1]
                    S.op("dve", RED(sm.ap[:, 0:1], sc.ap[:, lo:768], ALU.max), R=[sc.reg], W=[sm.reg])
                    S.op("dve", TS(sm.ap[:, 1:2], sm.ap[:, 0:1], skh, -1.0, ALU.max, ALU.mult),
                         R=[sm.reg, constR], W=[sm.reg])
                    pf = PF[pi % 2]
                    S.op("act", ACTF(pf.ap[:, 0:nk], sc.ap[:, lo:768], AF.Exp,
                                     bias=sm.ap[:, 1:2], accum_out=sm.ap[:, 2:3]),
                         R=[sc.reg, sm.reg], W=[pf.reg, sm.reg])
                    sm2 = SM.next()
                    S.op("act", ACTF(sm2.ap[:, 0:1], skh, AF.Exp, bias=sm.ap[:, 1:2]), R=[sm.reg, constR], W=[sm2.reg])
                    S.op("dve", TT(sm2.ap[:, 1:2], sm2.ap[:, 0:1], sm.ap[:, 2:3], ALU.add), R=[sm.reg, sm2.reg], W=[sm2.reg])
                    S.op("dve", RECIP(sm2.ap[:, 3:4], sm2.ap[:, 1:2]), R=[sm2.reg], W=[sm2.reg])
                    pn = PN[pi % 3]
                    S.op("dve", TS(pn.ap[:, 0:nk], pf.ap[:, 0:nk], sm2.ap[:, 3:4], None, ALU.mult),
                         R=[pf.reg, sm2.reg], W=[pn.reg])
                    st[pi] = (nk, nb, kb0, kb1)

                def stage2(pi):
                    nk, nb, kb0, kb1 = st[pi]
                    pn, ptp, pt = PN[pi % 3], PTP[pi % 2], PTs[pi % 3]
                    S.op("pe", [TR(ptp.ap[:, jb * 128:(jb + 1) * 128], pn.ap[:, jb * 128:(jb + 1) * 128], IDB[:])
                                for jb in range(nk // 128)], R=[pn.reg, constR], W=[ptp.reg])
                    S.op("act", ACTF(pt.ap[:, 0:nk], ptp.ap[:, 0:nk], AF.Copy), R=[ptp.reg], W=[pt.reg])

                def stage3(pi):
                    lc, qb, hh = pairs[pi]
                    nk, nb, kb0, kb1 = st[pi]
                    p0 = hh * 64
                    pt = PTs[pi % 3]
                    ob = OB[(pi // 2) % 2]
                    gq = min(qb // 4, 4)
                    tiles = (list(range(kb0, kb1 + 1)) if nb else []) + [16, 17]
                    S.op("pe", [MM(ob.ap[p0:p0 + 64, 0:128], Vt[:, tiles[jb], kvg * 64:(kvg + 1) * 64],
                                   pt.ap[:, jb * 128:(jb + 1) * 128], jb == 0, jb == len(tiles) - 1)
                                for jb in range(len(tiles))],
                         R=[pt.reg] + [Vr[i] for i in tiles], W=[ob.reg])
                    if hh == 1:
                        S.op("act", ACTF(Ot[:, lc, qb * 128:(qb + 1) * 128], ob.ap[:, 0:128], AF.Copy),
                             R=[ob.reg], W=[Or[lc][gq]])

                for step in range(len(pairs) + 2):
                    if step < len(pairs):
                        stage1(step)
                    if 0 <= step - 1 < len(pairs):
                        stage2(step - 1)
                    if 0 <= step - 2 < len(pairs):
                        stage3(step - 2)
                S.barrier()
                for m in range(KC):
                    for g in GO:
                        t0, n = TGS[g]
                        pb = MMB.next()
                        S.op("pe", [MM(pb.ap[:, :n], Xv[:, 2 * kvg + kk, m * 128:(m + 1) * 128],
                                       Ot[:, kk, t0:t0 + n], kk == 0, kk == 1) for kk in range(2)],
                             R=Xr + [Or[0][g], Or[1][g]], W=[pb.reg])
                        accum_H(l, 2, m, g, pb, n)
            S.barrier()

    def gates_compute(nt):
        bc = lambda ap: ap.unsqueeze(2).to_broadcast([128, nt, NEXP])
        gtb = TMP.next()
        gtR = gtb.reg
        GT = gtb.ap[:, 0:4 * NTILE * NEXP].rearrange("p (a t e) -> p a t e", a=4, t=NTILE)
        m1, m2, den = GT[:, 0, 0:nt, 0], GT[:, 0, 0:nt, 1], GT[:, 0, 0:nt, 2]
        e1, e2, e3 = GT[:, 1, 0:nt], GT[:, 2, 0:nt], GT[:, 3, 0:nt]
        LGv, GAv = LG[:, 0:nt], GATES[:, 0:nt]
        S.op("dve", RED(m1, LGv, ALU.max), R=[lgR], W=[gtR])
        S.op("dve", TT(e1, LGv, bc(m1), ALU.is_equal), R=[lgR, gtR], W=[gtR])
        S.op("dve", STT(e1, e1, -1e30, LGv, ALU.mult, ALU.add), R=[gtR, lgR], W=[gtR])
        S.op("dve", RED(m2, e1, ALU.max), R=[gtR], W=[gtR])
        S.op("dve", TT(e2, LGv, bc(m2), ALU.is_ge), R=[lgR, gtR], W=[gtR])
        S.op("dve", TT(e3, LGv, bc(m1), ALU.subtract), R=[lgR, gtR], W=[gtR])
        S.op("act", ACTF(e3, e3, AF.Exp), R=[gtR], W=[gtR])
        S.op("dve", TT(e2, e2, e3, ALU.mult), R=[gtR], W=[gtR])
        S.op("dve", RED(den, e2, ALU.add), R=[gtR], W=[gtR])
        S.op("dve", RECIP(den, den), R=[gtR], W=[gtR])
        S.op("dve", TT(GAv, e2, bc(den), ALU.mult), R=[gtR], W=[gatesR])

    BR3 = Ring([BB[1], BB[2], BB[3]])

    def ffn(l, side=()):
        side = list(side)
        moe = (l % 2 == 1)
        i = l // 2
        nexp = NEXP if moe else 1
        nch = (DFF_MOE if moe else DFF_DENSE) // 128
        GO = range(4) if l == DEPTH - 1 else range(5)
        if moe:
            gates_compute(16 if l == DEPTH - 1 else NTILE)
        with ExitStack() as ph:
            Gt = sb("G", [128, 4, T], BF16, ph)
            Gr_ = [[Reg("g%d_%d" % (j, g)) for g in range(5)] for j in range(4)]
            if moe:
                GBC = sb("GBC", [128, T], F32, ph)
                gbcR = [Reg("gbc%d" % g) for g in range(5)]
            for e in range(nexp):
                if moe:
                    w1s, w3s, w2s = mw1[i, e], mw3[i, e], mw2[i, e]
                    for g in GO:
                        t0, n = TGS[g]
                        pb = BR3.next()
                        for ii in range(n // 128):
                            ti = t0 // 128 + ii
                            gb = TMP.next()
                            S.op("dve", COPY(gb.ap[:, 0:128], GATES[:, ti, e:e + 1].to_broadcast([128, 128])),
                                 R=[gatesR], W=[gb.reg])
                            S.op("pe", MM(pb.ap[:, ii * 128:(ii + 1) * 128], gb.ap[:, 0:128], IDF[:]),
                                 R=[gb.reg, constR], W=[pb.reg])
                        S.op("act", ACTF(GBC[:, t0:t0 + n], pb.ap[:, :n], AF.Copy), R=[pb.reg], W=[gbcR[g]])
                else:
                    w1s, w3s, w2s = dw1[i], dw3[i], dw2[i]
                for c0 in range(0, nch, 4):
                    nj = min(4, nch - c0)
                    blk = []
                    for half in range((nj + 1) // 2):
                        s = next_slot()
                        si = (slot_i[0] - 1) % NSLOT
                        col = (c0 + 2 * half) * 128
                        v1 = RINGT[:, si, 0:2048].rearrange("p (k n) -> p k n", k=KC)
                        v3 = RINGT[:, si, 2048:4096].rearrange("p (k n) -> p k n", k=KC)
                        v2 = RINGT[:, si, 4096:6144].rearrange("p (j n) -> p j n", j=2)
                        S.dma("pool", v1, w1s[:, col:col + 256].rearrange("(k p) n -> p k n", p=128), W=[s[0].reg])
                        S.dma("pool", v3, w3s[:, col:col + 256].rearrange("(k p) n -> p k n", p=128), W=[s[1].reg])
                        S.dma("pool", v2, w2s[col:col + 256, :].rearrange("(j p) n -> p j n", p=128), W=[s[2].reg])
                        blk.append((v1, v3, v2, s))
                    for j in range(nj):
                        v1, v3, v2, s = blk[j // 2]
                        jc = (j % 2) * 128
                        for g in GO:
                            t0, n = TGS[g]
                            p1, p3 = MMB.next(), MMB.next()
                            S.op("pe", [MM(p1.ap[:, :n], v1[:, k, jc:jc + 128], A[:, k, t0:t0 + n],
                                           k == 0, k == KC - 1) for k in range(KC)],
                                 R=[s[0].reg] + [Ar[k][g] for k in range(KC)], W=[p1.reg])
                            S.op("pe", [MM(p3.ap[:, :n], v3[:, k, jc:jc + 128], A[:, k, t0:t0 + n],
                                           k == 0, k == KC - 1) for k in range(KC)],
                                 R=[s[1].reg] + [Ar[k][g] for k in range(KC)], W=[p3.reg])
                            sl = TMP.next()
                            S.op("act", ACTF(sl.ap[:, :n], p1.ap[:, :n], AF.Silu), R=[p1.reg], W=[sl.reg])
                            if moe:
                                tg_ = TMP.next()
                                S.op("dve", TT(tg_.ap[:, :n], p3.ap[:, :n], GBC[:, t0:t0 + n], ALU.mult),
                                     R=[p3.reg, gbcR[g]], W=[tg_.reg])
                                S.op("dve", TT(Gt[:, j, t0:t0 + n], tg_.ap[:, :n], sl.ap[:, :n], ALU.mult),
                                     R=[tg_.reg, sl.reg], W=[Gr_[j][g]])
                            else:
                                S.op("dve", TT(Gt[:, j, t0:t0 + n], p3.ap[:, :n], sl.ap[:, :n], ALU.mult),
                                     R=[p3.reg, sl.reg], W=[Gr_[j][g]])
                    for m in range(KC):
                        for g in GO:
                            t0, n = TGS[g]
                            pb = BR3.next()
                            S.op("pe", [MM(pb.ap[:, :n], blk[j // 2][2][:, j % 2, m * 128:(m + 1) * 128],
                                           Gt[:, j, t0:t0 + n], j == 0, j == nj - 1) for j in range(nj)],
                                 R=[b_[3][2].reg for b_ in blk] + [Gr_[j][g] for j in range(nj)], W=[pb.reg])
                            accum_H(l, 5, m, g, pb, n)
                    for _ in range(1 if moe else 2):
                        if side:
                            side.pop(0)()
            while side:
                side.pop(0)()
            S.barrier()

    def dump(src_is_A=False):
        for k in range(KC):
            S.dma("sp", dbg_d[k * 128:(k + 1) * 128, :], H[:, k, :], R=Hr[k], sem_reg=outR)

    outR = Reg("out")
    layers = list(layers)
    run_ln_jobs(modulate_jobs(layers[0], 0, first=(layers[0] == 0)))
    for li, l in enumerate(layers):
        GO = range(4) if l == DEPTH - 1 else range(5)
        token_mixer(l)
        if dbg == ("mix", l):
            break
        run_ln_jobs(postnorm_jobs(l, 0, groups=GO)
                    + modulate_jobs(l, 1, router_i=(l // 2 if l % 2 == 1 else None), groups=GO))
        ffn(l, side=([st_ for l2 in layers[li + 1:li + 3] for st_ in mods_steps(l2)] if (li == 1) else ()))
        if dbg == ("ffn", l):
            break
        nxt = modulate_jobs(layers[li + 1], 0) if li + 1 < len(layers) else []
        run_ln_jobs(postnorm_jobs(l, 1, groups=GO) + nxt)
    if dbg is not None:
        dump()
    for k in range(KC):
        S.dma("sp", outT[k * 128:(k + 1) * 128, :], H[:, k, 0:NLAT], R=Hr[k][0:4], sem_reg=outR)
    S.eng["sp"].h.wait_ge(outR.dsem, outR.dcnt)
    es.close()
    return nc


def _host_consts():
    freqs = 10000.0 ** (-np.arange(16, dtype=np.float32) / 16)
    t = np.arange(NLAT)
    row = (t // 64).astype(np.float32)
    col = (t % 64).astype(np.float32)
    cos = np.zeros((128, NLAT), np.float32)
    sin = np.zeros((128, NLAT), np.float32)
    for p in range(128):
        d = p % 64
        blk = d // 16
        pos = row if blk < 2 else col
        ang = pos * freqs[d % 16]
        cos[p] = np.cos(ang)
        sin[p] = np.sin(ang) * (-1.0 if blk % 2 == 0 else 1.0)
    tabs = np.stack([cos.reshape(128, 4, 512), sin.reshape(128, 4, 512)], axis=2)
    qi = np.arange(128)[:, None]
    j = np.arange(128)[None, :]
    mask = np.zeros((128, 384), np.float32)
    mask[:, 0:128] = np.where(j >= qi, 0.0, MASKNEG)
    mask[:, 256:384] = np.where(j <= qi, 0.0, MASKNEG)
    edge = np.zeros((128, 4, 2, 8), np.float32)
    for gi, w in enumerate(POOL_WINDOWS):
        hw = w // 2
        for tt in range(hw):
            edge[:, gi, 0, tt] = 1.0 / (tt + hw)
        for ii in range(hw - 1):
            edge[:, gi, 1, ii] = 1.0 / (w - 1 - ii)
    return dict(
        tabs=np.ascontiguousarray(tabs),
        mask=mask.astype(ml_dtypes.bfloat16),
        identb=np.eye(128, dtype=np.float32).astype(ml_dtypes.bfloat16),
        identf=np.eye(128, dtype=np.float32),
        onesf=np.ones((128, 128), np.float32),
        onesd=np.full((128, 128), 1.0 / D, np.float32),
        epsc=np.tile(np.array([[LN_EPS, LN_EPS * ALPHA * ALPHA]], np.float32), (128, 1)),
        edge=edge.reshape(128, 64),
    )


def _win_ext_cols():
    def partner(cols):
        out = []
        for c in cols:
            d = c % 64
            out.append(c - d + (d + 16 if (d // 16) % 2 == 0 else d - 16))
        return out
    cols = list(range(768, 1280)) + list(range(640, 768))
    for kvg in range(2):
        q = list(range(kvg * 256, kvg * 256 + 256))
        kk = list(range(512 + kvg * 64, 512 + kvg * 64 + 64)) * 2
        cols += q + partner(q) + kk + partner(kk)
    assert len(cols) == WIN_EXT
    return np.array(cols)


def _prep_inputs(inp):
    f = lambda a: np.ascontiguousarray(np.asarray(a, dtype=np.float32))
    common = _host_consts()
    fm = lambda v: f(v).reshape(-1, 128).T
    common["badaT"] = np.ascontiguousarray(np.stack([fm(inp["b_ada"][l]) for l in range(DEPTH)], axis=1))
    lngb = np.zeros((128, DEPTH, 2, 2, KC), np.float32)
    for l in range(DEPTH):
        for s_ in range(2):
            lngb[:, l, s_, 0] = fm(inp["ln_g"][l, s_])
            lngb[:, l, s_, 1] = fm(inp["ln_b"][l, s_])
    common["lngb"] = lngb.reshape(128, -1)
    common["pscale"] = np.ascontiguousarray(np.stack([fm(inp["pool_scale"][l]) for l in range(DEPTH)], axis=1)).reshape(128, -1)
    common["sinkb"] = np.ascontiguousarray(np.broadcast_to(f(inp["sink"]).reshape(1, -1), (128, DEPTH * 8)))
    r = f(inp["router"]).reshape(2, KC, 128, NEXP).transpose(2, 0, 1, 3)
    common["routerT"] = np.ascontiguousarray(r).reshape(128, -1)
    common["w_ada"] = f(inp["w_ada"])
    common["w_inx"] = np.ascontiguousarray(f(inp["w_in"])[:, :, _win_ext_cols()])
    for k in ("w_pool", "w_out", "dense_w1", "dense_w3", "dense_w2", "moe_w1", "moe_w3", "moe_w2"):
        common[k] = f(inp[k])
    x, c, ctx, c_ctx = f(inp["x"]), f(inp["c"]), f(inp["ctx"]), f(inp["c_ctx"])
    maps = []
    for b in range(8):
        m = dict(common)
        m["xT"] = np.ascontiguousarray(x[b].T)
        m["ctxT"] = np.ascontiguousarray(ctx[b].T)
        m["cc"] = np.ascontiguousarray(np.stack([fm(c[b]), fm(c_ctx)], axis=2))
        maps.append(m)
    return maps


def kernel(**inputs):
    maps = _prep_inputs(inputs)
    nc = build()
    res = run_bass_kernel_spmd(nc, maps, core_ids=list(range(8)))
    out = np.stack([np.asarray(r["outT"]).T for r in res.results], axis=0)
    return np.ascontiguousarray(out.astype(np.float32))
```

```python
import numpy as np
import ml_dtypes
from contextlib import ExitStack
import concourse.bass as bass
import concourse.mybir as mybir
from concourse.bass_utils import run_bass_kernel_spmd

F32 = mybir.dt.float32
BF16 = mybir.dt.bfloat16
AF = mybir.ActivationFunctionType
ALU = mybir.AluOpType
AX = mybir.AxisListType

D = 1024
KC = 8
NLAT = 2048
NCTX = 256
T = NLAT + NCTX
DEPTH = 4
TGS = [(0, 512), (512, 512), (1024, 512), (1536, 512), (2048, 256)]
NTILE = T // 128
POOL_WINDOWS = (2, 4, 8, 16)
DFF_DENSE = 2816
DFF_MOE = 3584
NEXP = 8
ALPHA = (2 * DEPTH) ** 0.25
LN_EPS = 1e-6
LP = 2336
ULAT = 8
UCTX = 2072
MASKNEG = -240000.0
SLOT = 6144
NSLOT = 3
WIN_EXT = 2176


class Tok:
    __slots__ = ("key", "sem", "val", "clock")

    def __init__(self, key, sem, val, clock):
        self.key, self.sem, self.val, self.clock = key, sem, val, clock


class Reg:
    __slots__ = ("name", "w", "r", "dsem", "dcnt")

    def __init__(self, name):
        self.name, self.w, self.r, self.dsem, self.dcnt = name, None, {}, None, 0


class Eng:
    def __init__(self, name, h, sem):
        self.name, self.h, self.sem, self.cnt, self.seen, self.last = name, h, sem, 0, {}, None
        self.pending = []


class Sched:
    def __init__(self, nc, es):
        self.nc, self.es = nc, es
        self.eng = {}
        for name, h in (("pe", nc.tensor), ("act", nc.scalar), ("dve", nc.vector),
                        ("pool", nc.gpsimd), ("sp", nc.sync)):
            self.eng[name] = Eng(name, h, es.enter_context(nc.semaphore("sem_" + name)))
        self.nsem = 0

    def _deps(self, E, R, W):
        deps = list(E.pending)
        E.pending = []
        for r in R:
            if r.w is not None:
                deps.append(r.w)
        for w in W:
            if w.w is not None and (w.w.key != E.name or E.name != "pe"):
                deps.append(w.w)
            for k, t in w.r.items():
                if k != E.name or E.name != "pe":
                    deps.append(t)
        return deps

    def _wait(self, E, deps):
        for t in deps:
            if E.seen.get(t.key, 0) >= t.val:
                continue
            E.h.wait_ge(t.sem, t.val)
            E.seen[t.key] = t.val
            for k, v in t.clock.items():
                if E.seen.get(k, 0) < v:
                    E.seen[k] = v

    def op(self, en, fns, R=(), W=()):
        E = self.eng[en]
        self._wait(E, self._deps(E, R, W))
        if callable(fns):
            fns = [fns]
        ins = None
        for f in fns:
            ins = f(E.h)
        E.cnt += 1
        ins.then_inc(E.sem, 1)
        clock = dict(E.seen)
        clock[E.name] = E.cnt
        t = Tok(E.name, E.sem, E.cnt, clock)
        E.last = t
        for r in R:
            r.r[E.name] = t
        for w in W:
            w.w = t
            w.r = {}
        return t

    def dma(self, qn, out, in_, R=(), W=(), sem_reg=None):
        E = self.eng[qn]
        self._wait(E, self._deps(E, R, W))
        reg = sem_reg if sem_reg is not None else W[0]
        if reg.dsem is None:
            reg.dsem = self.es.enter_context(self.nc.semaphore("dsem%d" % self.nsem))
            self.nsem += 1
        reg.dcnt += 16
        E.h.dma_start(out=out, in_=in_).then_inc(reg.dsem, 16)
        t = Tok(("d", id(reg)), reg.dsem, reg.dcnt, dict(E.seen))
        for r in R:
            r.r[t.key] = t
        for w in W:
            w.w = t
            w.r = {}
        return t

    def barrier(self, names=("pe", "act", "dve", "sp")):
        toks = [self.eng[n].last for n in names if self.eng[n].last is not None]
        for n in names:
            self.eng[n].pending = [t for t in toks if t.key != n]


class Buf:
    __slots__ = ("ap", "reg")

    def __init__(self, ap, name):
        self.ap, self.reg = ap, Reg(name)


class Ring:
    def __init__(self, bufs):
        self.bufs, self.i = bufs, 0

    def next(self):
        b = self.bufs[self.i % len(self.bufs)]
        self.i += 1
        return b


def MM(out, lhsT, rhs, st=True, sp=True):
    return lambda h: h.matmul(out, lhsT, rhs, start=st, stop=sp)


def TR(out, in_, ident):
    return lambda h: h.transpose(out, in_, ident)


def ACTF(out, in_, func, **kw):
    return lambda h: h.activation(out=out, in_=in_, func=func, **kw)


def TT(out, a, b, op):
    return lambda h: h.tensor_tensor(out=out, in0=a, in1=b, op=op)


def TS(out, a, s1, s2, op0, op1=None):
    if op1 is None:
        return lambda h: h.tensor_scalar(out=out, in0=a, scalar1=s1, scalar2=None, op0=op0)
    return lambda h: h.tensor_scalar(out=out, in0=a, scalar1=s1, scalar2=s2, op0=op0, op1=op1)


def STT(out, a, s, b, op0, op1):
    return lambda h: h.scalar_tensor_tensor(out=out, in0=a, scalar=s, in1=b, op0=op0, op1=op1)


def RED(out, in_, op, axis=None):
    return lambda h: h.tensor_reduce(out=out, in_=in_, axis=(axis or AX.X), op=op)


def RECIP(out, in_):
    return lambda h: h.reciprocal(out=out, in_=in_)


def COPY(out, in_):
    return lambda h: h.tensor_copy(out=out, in_=in_)


def MEMSET(ap, v):
    return lambda h: h.memset(ap, v)


def build(layers=(0, 1, 2, 3), dbg=None):
    nc = bass.Bass("TRN2", target_bir_lowering=False)
    es = ExitStack()

    def din(name, shape, dt=F32):
        return nc.dram_tensor(name, list(shape), dt, kind="ExternalInput").ap()

    xT = din("xT", [D, NLAT])
    ctxT = din("ctxT", [D, NCTX])
    cc_d = din("cc", [128, KC, 2])
    bada_d = din("badaT", [128, DEPTH, 48])
    lngb_d = din("lngb", [128, DEPTH * 2 * 2 * KC])
    psc_d = din("pscale", [128, DEPTH * 4])
    sink_d = din("sinkb", [128, DEPTH * 8])
    rout_d = din("routerT", [128, 2 * KC * NEXP])
    tabs_d = din("tabs", [128, 4, 2, 512])
    mask_d = din("mask", [128, 384], BF16)
    idb_d = din("identb", [128, 128], BF16)
    idf_d = din("identf", [128, 128])
    onesf_d = din("onesf", [128, 128])
    edge_d = din("edge", [128, 4 * 2 * 8])
    eps_d = din("epsc", [128, 2])
    onesd_d = din("onesd", [128, 128])
    w_ada = din("w_ada", [DEPTH, D, 6 * D])
    w_inx = din("w_inx", [DEPTH, D, WIN_EXT])
    w_pool = din("w_pool", [DEPTH, 4, 128, 128])
    w_out = din("w_out", [DEPTH, D, D])
    dw1 = din("dense_w1", [2, D, DFF_DENSE])
    dw3 = din("dense_w3", [2, D, DFF_DENSE])
    dw2 = din("dense_w2", [2, DFF_DENSE, D])
    mw1 = din("moe_w1", [2, NEXP, D, DFF_MOE])
    mw3 = din("moe_w3", [2, NEXP, D, DFF_MOE])
    mw2 = din("moe_w2", [2, NEXP, DFF_MOE, D])
    outT = nc.dram_tensor("outT", [D, NLAT], F32, kind="ExternalOutput").ap()
    if dbg is not None:
        dbg_d = nc.dram_tensor("dbg", [D, T], F32, kind="ExternalOutput").ap()

    S = Sched(nc, es)

    uniq = [0]

    def sb(name, shape, dt=F32, st=es):
        uniq[0] += 1
        return st.enter_context(nc.sbuf_tensor("%s_%d" % (name, uniq[0]), list(shape), dt))

    def ps(name, shape, dt=F32):
        return es.enter_context(nc.psum_tensor(name, list(shape), dt))

    H = sb("H", [128, KC, T])
    A = sb("A", [128, KC, T], BF16)
    Hr = [[Reg("H%d_%d" % (k, g)) for g in range(5)] for k in range(KC)]
    Ar = [[Reg("A%d_%d" % (k, g)) for g in range(5)] for k in range(KC)]
    RINGT = sb("ring", [128, NSLOT, SLOT], BF16)
    slots = [[Buf(RINGT[:, s, j * 2048:(j + 1) * 2048], "slot%d_%d" % (s, j)) for j in range(3)]
             for s in range(NSLOT)]
    slot_i = [0]
    WP = sb("wp", [128, 1, 4, 128], BF16)
    WPb = [Buf(WP[:, 0], "wp0")] * 2
    TMPT = sb("tmp", [128, 5, 644])
    TMP = Ring([Buf(TMPT[:, i], "tmp%d" % i) for i in range(5)])
    STT_ = sb("stat", [128, 3, 512])
    STAT = [Buf(STT_[:, i], "stat%d" % i) for i in range(3)]
    SMT = sb("small", [128, 8, 4])
    SM = Ring([Buf(SMT[:, i], "sm%d" % i) for i in range(8)])
    CC = sb("ccs", [128, KC, 2])
    CCB = sb("ccb", [128, KC, 2], BF16)
    BADA = sb("bada", [128, DEPTH, 48])
    MOD = sb("mod", [128, DEPTH, 48, 2])
    LNGB = sb("lngbs", [128, DEPTH * 2 * 2 * KC])
    PSC = sb("pscs", [128, DEPTH * 4])
    SK = sb("sk", [128, DEPTH * 8])
    ROUT = sb("routs", [128, 2 * KC * NEXP])
    MASK = sb("masks", [128, 384], BF16)
    IDB = sb("idb", [128, 128], BF16)
    IDF = sb("idf", [128, 128])
    ONESF = sb("onesfs", [128, 128])
    EDGE = sb("edges", [128, 4 * 2 * 8])
    EPSC = sb("epscs", [128, 2])
    ONESD = sb("onesds", [128, 128])
    LG = sb("lg", [128, NTILE, NEXP])
    GATES = sb("gates", [128, NTILE, NEXP])
    Vt = sb("V", [128, NTILE, 128], BF16)
    constR = Reg("const")
    modRs = [Reg("mod%d" % i) for i in range(DEPTH)]
    ccR = Reg("cc")
    lgR = Reg("lg")
    gatesR = Reg("gates")
    gtR = Reg("gt")
    Vr = [Reg("V%d" % i) for i in range(NTILE)]

    S0 = ps("S0", [128, 1024])
    S1 = ps("S1", [128, 1024])
    B = [ps("B%d" % i, [128, 512]) for i in range(4)]
    SC = [Buf(S0, "S0"), Buf(S1, "S1")]
    MMB = Ring([Buf(S0[:, 0:512], "mm0"), Buf(S0[:, 512:1024], "mm1"),
                Buf(S1[:, 0:512], "mm2"), Buf(S1[:, 512:1024], "mm3")])
    MMB.bufs[0].reg = MMB.bufs[1].reg = SC[0].reg
    MMB.bufs[2].reg = MMB.bufs[3].reg = SC[1].reg
    MMB.bufs = [MMB.bufs[0], MMB.bufs[2], MMB.bufs[1], MMB.bufs[3]]
    BB = [Buf(B[i], "B%d" % i) for i in range(4)]
    BR = Ring(BB)

    def mod_ap(l, which, k, s):
        return MOD[:, l, which * 8 + k, s:s + 1]

    def tgs_of(g):
        return 0 if g < 4 else 1

    for dst, src in ((CC, cc_d), (BADA, bada_d), (LNGB, lngb_d), (PSC, psc_d), (SK, sink_d),
                     (ROUT, rout_d), (MASK, mask_d), (IDB, idb_d), (IDF, idf_d),
                     (ONESF, onesf_d), (EDGE, edge_d), (EPSC, eps_d), (ONESD, onesd_d)):
        S.dma("sp", dst[:], src, W=[constR])
    for k in range(KC):
        S.dma("sp", H[:, k, 0:NLAT], xT[k * 128:(k + 1) * 128, :], W=Hr[k][0:4])
        S.dma("sp", H[:, k, NLAT:T], ctxT[k * 128:(k + 1) * 128, :], W=[Hr[k][4]])
    S.op("act", ACTF(CCB[:], CC[:], AF.Silu), R=[constR], W=[ccR])
    S.op("dve", TS(LNGB[:, 0:112], LNGB[:, 0:112], float(ALPHA), None, ALU.mult), R=[constR], W=[constR])

    def next_slot():
        s = slots[slot_i[0] % NSLOT]
        slot_i[0] += 1
        return s

    def load_slot(src_ap, ncols, kdim=KC):
        s = next_slot()
        base = s[0].ap.tensor if False else None
        si = (slot_i[0] - 1) % NSLOT
        view = RINGT[:, si, 0:kdim * ncols].rearrange("p (k n) -> p k n", k=kdim)
        S.dma("pool", view, src_ap.rearrange("(k p) n -> p k n", p=128),
              W=[s[0].reg, s[1].reg, s[2].reg])
        return view, [s[0].reg, s[1].reg, s[2].reg]

    def mods_steps(l):
        pm = BB[0]
        steps = []

        def piece(sidx):
            view, regs = load_slot(w_ada[l, :, sidx * 768:(sidx + 1) * 768], 768)
            for jj in range(6):
                j = sidx * 6 + jj
                S.op("pe", [MM(pm.ap[:, 2 * j:2 * j + 2], view[:, k, jj * 128:(jj + 1) * 128],
                               CCB[:, k, :], k == 0, k == KC - 1) for k in range(KC)],
                     R=regs + [ccR], W=[pm.reg])

        def finish():
            for s_ in range(2):
                S.op("dve", TT(MOD[:, l, :, s_], pm.ap[:, 0:96].rearrange("p (j s) -> p j s", s=2)[:, :, s_],
                               BADA[:, l, :], ALU.add), R=[pm.reg, constR], W=[modRs[l]])
            for which in (1, 4):
                S.op("dve", TS(MOD[:, l, which * 8:(which + 1) * 8, :], MOD[:, l, which * 8:(which + 1) * 8, :],
                               1.0, None, ALU.add), R=[modRs[l]], W=[modRs[l]])

        for sidx in range(8):
            steps.append(lambda sidx=sidx: piece(sidx))
        steps.append(finish)
        return steps

    for l_ in list(layers)[:2]:
        for st_ in mods_steps(l_):
            st_()

    stat_i = [0]

    def ln_A(g):
        t0, n = TGS[g]
        pa, pb = (BB[0], BB[1]) if stat_i[0] % 2 == 0 else (BB[2], BB[3])
        stat_i[0] += 1
        for k in range(KC):
            sq = TMP.next()
            S.op("pool", TT(sq.ap[:, :n], H[:, k, t0:t0 + n], H[:, k, t0:t0 + n], ALU.mult), R=[Hr[k][g]], W=[sq.reg])
            S.op("pe", [MM(pa.ap[:, :n], ONESD[:], H[:, k, t0:t0 + n], k == 0, k == KC - 1),
                        MM(pb.ap[:, :n], ONESD[:], sq.ap[:, :n], k == 0, k == KC - 1)],
                 R=[Hr[k][g], sq.reg, constR], W=[pa.reg, pb.reg])
        return pa, pb

    def ln_chain(g, pa, pb, epsi):
        t0, n = TGS[g]
        msq, rstd, nmr = STAT
        S.op("act", ACTF(msq.ap[:, :n], pa.ap[:, :n], AF.Square), R=[pa.reg], W=[msq.reg])
        S.op("dve", TT(rstd.ap[:, :n], pb.ap[:, :n], msq.ap[:, :n], ALU.subtract), R=[pb.reg, msq.reg], W=[rstd.reg])
        S.op("act", ACTF(rstd.ap[:, :n], rstd.ap[:, :n], AF.Sqrt, bias=EPSC[:, epsi:epsi + 1]),
             R=[rstd.reg, constR], W=[rstd.reg])
        S.op("dve", RECIP(rstd.ap[:, :n], rstd.ap[:, :n]), R=[rstd.reg], W=[rstd.reg])
        S.op("dve", STT(nmr.ap[:, :n], pa.ap[:, :n], -1.0, rstd.ap[:, :n], ALU.mult, ALU.mult),
             R=[pa.reg, rstd.reg], W=[nmr.reg])

    def ln_apply(g, emit):
        t0, n = TGS[g]
        msq, rstd, nmr = STAT
        for k0 in range(0, KC, 2):
            xs = [TMP.next(), TMP.next()]
            for j in range(2):
                S.op("dve", TT(xs[j].ap[:, :n], H[:, k0 + j, t0:t0 + n], rstd.ap[:, :n], ALU.mult),
                     R=[Hr[k0 + j][g], rstd.reg], W=[xs[j].reg])
            for j in range(2):
                S.op("dve", TT(xs[j].ap[:, :n], xs[j].ap[:, :n], nmr.ap[:, :n], ALU.add),
                     R=[xs[j].reg, nmr.reg], W=[xs[j].reg])
            for j in range(2):
                emit(k0 + j, xs[j], n)

    def run_ln_jobs(jobs):
        stats = ln_A(jobs[0][0])
        for ji, (g, emit, epsi, post) in enumerate(jobs):
            pa, pb = stats
            ln_chain(g, pa, pb, epsi)
            if ji + 1 < len(jobs):
                stats = ln_A(jobs[ji + 1][0])
            ln_apply(g, emit)
            if post is not None:
                post()

    RL = Buf(S0[:, 0:512], "rl")
    RL.reg = SC[0].reg

    def modulate_jobs(l, sub, router_i=None, groups=range(5), first=False):
        wsh, wsc = (0, 1) if sub == 0 else (3, 4)
        jobs = []
        for g in groups:
            t0, n = TGS[g]
            s_ = tgs_of(g)
            rl = RL

            def emit(k, xh, n, g=g, t0=t0, s_=s_, rl=rl):
                if router_i is None:
                    S.op("act", ACTF(A[:, k, t0:t0 + n], xh.ap[:, :n], AF.Identity,
                                     scale=mod_ap(l, wsc, k, s_), bias=mod_ap(l, wsh, k, s_)),
                         R=[xh.reg, modRs[l]], W=[Ar[k][g]])
                else:
                    S.op("act", ACTF(xh.ap[:, :n], xh.ap[:, :n], AF.Identity,
                                     scale=mod_ap(l, wsc, k, s_), bias=mod_ap(l, wsh, k, s_)),
                         R=[xh.reg, modRs[l]], W=[xh.reg])
                    S.op("act", ACTF(A[:, k, t0:t0 + n], xh.ap[:, :n], AF.Copy), R=[xh.reg], W=[Ar[k][g]])
                    S.op("pe", MM(rl.ap[0:NEXP, :n],
                                  ROUT[:, (router_i * KC + k) * NEXP:(router_i * KC + k + 1) * NEXP],
                                  xh.ap[:, :n], k == 0, k == KC - 1), R=[xh.reg, constR], W=[rl.reg])
                if first:
                    S.op("act", ACTF(H[:, k, t0:t0 + n], H[:, k, t0:t0 + n], AF.Copy, scale=float(ALPHA)),
                         R=[Hr[k][g]], W=[Hr[k][g]])

            def post(t0=t0, n=n, rl=rl):
                lt = TMP.next()
                S.op("act", ACTF(lt.ap[0:NEXP, :n], rl.ap[0:NEXP, :n], AF.Copy), R=[rl.reg], W=[lt.reg])
                nt = n // 128
                S.op("pe", [MM(rl.ap[:, i * NEXP:(i + 1) * NEXP],
                               lt.ap[0:NEXP, i * 128:(i + 1) * 128], IDF[0:NEXP, 0:NEXP], True, True)
                            for i in range(nt)], R=[lt.reg, constR], W=[rl.reg])
                S.op("dve", COPY(LG[:, t0 // 128:t0 // 128 + nt, :],
                                 rl.ap[:, 0:nt * NEXP].rearrange("p (i e) -> p i e", e=NEXP)),
                     R=[rl.reg], W=[lgR])

            jobs.append((g, emit, (0 if first else 1), post if router_i is not None else None))
        return jobs

    def postnorm_jobs(l, sub, groups=range(5)):
        jobs = []
        for g in groups:
            t0, n = TGS[g]

            def emit(k, xh, n, g=g, t0=t0):
                gi = ((l * 2 + sub) * 2 + 0) * KC + k
                bi = ((l * 2 + sub) * 2 + 1) * KC + k
                S.op("act", ACTF(H[:, k, t0:t0 + n], xh.ap[:, :n], AF.Identity,
                                 scale=LNGB[:, gi:gi + 1], bias=LNGB[:, bi:bi + 1]),
                     R=[xh.reg, constR], W=[Hr[k][g]])

            jobs.append((g, emit, 0, None))
        return jobs

    def accum_H(l, which, m, g, pbuf, n):
        t0 = TGS[g][0]
        S.op("dve", STT(H[:, m, t0:t0 + n], pbuf.ap[:, :n], mod_ap(l, which, m, tgs_of(g)),
                        H[:, m, t0:t0 + n], ALU.mult, ALU.add),
             R=[pbuf.reg, modRs[l], Hr[m][g]], W=[Hr[m][g]])

    def tm_prefetch(l):
        Uv, Ur = load_slot(w_inx[l, :, 0:640], 640)
        wpb = WPb[l % 2]
        S.dma("pool", wpb.ap, w_pool[l].rearrange("g d e -> d g e"), W=[wpb.reg])
        Yv, Yr = load_slot(w_out[l, 512:1024, :], 1024, kdim=4)
        return Uv, Ur, wpb, Yv, Yr

    def token_mixer(l, pre):
        last = (l == DEPTH - 1)
        GO = range(4) if last else range(5)
        Uv, Ur, wpb, Yv, Yr = pre
        with ExitStack() as ph:
            Up = Buf(sb("Up", [128, LP], F32, ph), "Up")
            Sw = Buf(sb("Sw", [128, LP], F32, ph), "Sw")
            Dd = Buf(sb("Dd", [128, T], BF16, ph), "Dd")
            PMt = sb("PM", [128, 2, T], BF16, ph)
            PMr = [[Reg("pm%d_%d" % (i, g)) for g in range(5)] for i in range(2)]
            LPE = (ULAT + NLAT + 8) if last else LP
            seqs = ((ULAT, 0, NLAT),) if last else ((ULAT, 0, NLAT), (UCTX, NLAT, NCTX))
            pads = ((0, ULAT), (ULAT + NLAT, LPE)) if last else ((0, ULAT), (ULAT + NLAT, UCTX), (UCTX + NCTX, LP))
            for gi, w in enumerate(POOL_WINDOWS):
                hw = w // 2
                for lo, hi in pads:
                    S.op("dve", MEMSET(Up.ap[:, lo:hi], 0.0), W=[Up.reg])
                for g in GO:
                    t0, n = TGS[g]
                    pb = MMB.next()
                    S.op("pe", [MM(pb.ap[:, :n], Uv[:, k, gi * 128:(gi + 1) * 128], A[:, k, t0:t0 + n],
                                   k == 0, k == KC - 1) for k in range(KC)],
                         R=Ur + [Ar[k][g] for k in range(KC)], W=[pb.reg])
                    u0 = ULAT + t0 if g < 4 else UCTX
                    S.op("act", ACTF(Up.ap[:, u0:u0 + n], pb.ap[:, :n], AF.Copy), R=[pb.reg], W=[Up.reg])
                S.op("dve", TT(Sw.ap[:, 0:LPE - 1], Up.ap[:, 0:LPE - 1], Up.ap[:, 1:LPE], ALU.add),
                     R=[Up.reg], W=[Sw.reg])
                sh, ext = 2, LPE - 1
                while sh < w:
                    ext -= sh
                    S.op("dve", TT(Sw.ap[:, 0:ext], Sw.ap[:, 0:ext], Sw.ap[:, sh:sh + ext], ALU.add),
                         R=[Sw.reg], W=[Sw.reg])
                    sh *= 2
                for bu, bd, n in seqs:
                    S.op("dve", STT(Dd.ap[:, bd:bd + n], Sw.ap[:, bu - hw:bu - hw + n], 1.0 / w,
                                    Up.ap[:, bu:bu + n], ALU.mult, ALU.subtract),
                         R=[Sw.reg, Up.reg], W=[Dd.reg])
                    e0 = (gi * 2 + 0) * 8
                    tl = TMP.next()
                    S.op("dve", TT(tl.ap[:, 0:hw], Sw.ap[:, bu - hw:bu], EDGE[:, e0:e0 + hw], ALU.mult),
                         R=[Sw.reg, constR], W=[tl.reg])
                    S.op("dve", TT(Dd.ap[:, bd:bd + hw], tl.ap[:, 0:hw], Up.ap[:, bu:bu + hw], ALU.subtract),
                         R=[tl.reg, Up.reg], W=[Dd.reg])
                    if hw > 1:
                        e1 = (gi * 2 + 1) * 8
                        c0 = n - hw + 1
                        tr_ = TMP.next()
                        S.op("dve", TT(tr_.ap[:, 0:hw - 1], Sw.ap[:, bu + c0 - hw:bu + n - hw],
                                       EDGE[:, e1:e1 + hw - 1], ALU.mult), R=[Sw.reg, constR], W=[tr_.reg])
                        S.op("dve", TT(Dd.ap[:, bd + c0:bd + n], tr_.ap[:, 0:hw - 1], Up.ap[:, bu + c0:bu + n],
                                       ALU.subtract), R=[tr_.reg, Up.reg], W=[Dd.reg])
                for g in GO:
                    t0, n = TGS[g]
                    pb = MMB.next()
                    S.op("pe", MM(pb.ap[:, :n], wpb.ap[:, gi, :], Dd.ap[:, t0:t0 + n]),
                         R=[wpb.reg, Dd.reg], W=[pb.reg])
                    S.op("act", ACTF(PMt[:, gi % 2, t0:t0 + n], pb.ap[:, :n], AF.Identity,
                                     scale=PSC[:, l * 4 + gi:l * 4 + gi + 1]),
                         R=[pb.reg, constR], W=[PMr[gi % 2][g]])
                if gi % 2 == 1:
                    for m in range(KC):
                        for g in GO:
                            t0, n = TGS[g]
                            pb = MMB.next()
                            S.op("pe", [MM(pb.ap[:, :n], Yv[:, gi - 1 + kk, m * 128:(m + 1) * 128],
                                           PMt[:, kk, t0:t0 + n], kk == 0, kk == 1) for kk in range(2)],
                                 R=Yr + [PMr[0][g], PMr[1][g]], W=[pb.reg])
                            accum_H(l, 2, m, g, pb, n)
            for i in range(NTILE):
                g = min(i // 4, 4)
                pb = MMB.next()
                S.op("pe", [MM(pb.ap[:, 0:128], A[:, k, i * 128:(i + 1) * 128], Uv[:, k, 512:640],
                               k == 0, k == KC - 1) for k in range(KC)],
                     R=Ur + [Ar[k][g] for k in range(KC)], W=[pb.reg])
                S.op("act", ACTF(Vt[:, i, :], pb.ap[:, 0:128], AF.Copy), R=[pb.reg], W=[Vr[i]])
            S.barrier()
        with ExitStack() as ph:
            Qt = sb("Q", [128, 2, T], BF16, ph)
            Kt = sb("K", [128, T], BF16, ph)
            Ot = sb("O", [128, 2, T], BF16, ph)
            TABt = sb("TAB", [128, 2, 512], F32, ph)
            Qr = [[Reg("q%d_%d" % (c, g)) for g in range(5)] for c in range(2)]
            Kr = [Reg("k%d" % g) for g in range(5)]
            Or = [[Reg("o%d_%d" % (c, g)) for g in range(5)] for c in range(2)]
            tabR = Reg("tab")
            TF = TMPT[:].rearrange("p a b -> p (a b)")
            PF = [Buf(TF[:, i * 644:(i + 1) * 644], "pf%d" % i) for i in range(2)]
            TB = TF[:, 1288:3220].bitcast(BF16)
            PN = [Buf(TB[:, i * 640:(i + 1) * 640], "pn%d" % i) for i in range(3)]
            PTs = [Buf(TB[:, (3 + i) * 640:(4 + i) * 640], "pt%d" % i) for i in range(3)]
            PTP = [Buf(BB[i].ap.bitcast(BF16), "ptp%d" % i) for i in range(2)]
            PTP[0].reg, PTP[1].reg = BB[0].reg, BB[1].reg
            OB = [BB[2], BB[3]]
            Gs = [load_slot(w_inx[l, :, 640:640 + 768], 768)]
            Xv, Xr = load_slot(w_out[l, 0:512, :], 1024, kdim=4)
            Gs.append(load_slot(w_inx[l, :, 640 + 768:640 + 2 * 768], 768))
            nqb = 16 if last else NTILE
            for kvg in range(2):
                Gv, Gr = Gs[kvg]
                for g in range(5):
                    t0, n = TGS[g]
                    if g < 4:
                        S.dma("sp", TABt[:], tabs_d[:, g], W=[tabR])
                    for c in range(3):
                        if c < 2 and g == 4 and last:
                            continue
                        ca, cb = (c, c + 2) if c < 2 else (4, 5)
                        dst = Qt[:, c, t0:t0 + n] if c < 2 else Kt[:, t0:t0 + n]
                        dreg = Qr[c][g] if c < 2 else Kr[g]
                        qs = 0.125 if c < 2 else 1.0
                        pa = MMB.next()
                        S.op("pe", [MM(pa.ap[:, :n], Gv[:, k, ca * 128:(ca + 1) * 128], A[:, k, t0:t0 + n],
                                       k == 0, k == KC - 1) for k in range(KC)],
                             R=Gr + [Ar[k][g] for k in range(KC)], W=[pa.reg])
                        if g == 4:
                            S.op("act", ACTF(dst, pa.ap[:, :n], AF.Copy, scale=qs), R=[pa.reg], W=[dreg])
                            continue
                        pb = MMB.next()
                        S.op("pe", [MM(pb.ap[:, :n], Gv[:, k, cb * 128:(cb + 1) * 128], A[:, k, t0:t0 + n],
                                       k == 0, k == KC - 1) for k in range(KC)],
                             R=Gr + [Ar[k][g] for k in range(KC)], W=[pb.reg])
                        t1, t2 = TMP.next(), TMP.next()
                        S.op("dve", TT(t1.ap[:, :n], pa.ap[:, :n], TABt[:, 0, :n], ALU.mult),
                             R=[pa.reg, tabR], W=[t1.reg])
                        S.op("dve", STT(t2.ap[:, :n], pb.ap[:, :n], qs, TABt[:, 1, :n], ALU.mult, ALU.mult),
                             R=[pb.reg, tabR], W=[t2.reg])
                        S.op("dve", STT(dst, t1.ap[:, :n], qs, t2.ap[:, :n], ALU.mult, ALU.add),
                             R=[t1.reg, t2.reg], W=[dreg])
                S.barrier()
                pairs = [(lc, qb, hh) for lc in range(2) for qb in range(nqb) for hh in range(2)]
                st = {}

                def stage1(pi):
                    lc, qb, hh = pairs[pi]
                    head = kvg * 4 + lc * 2 + hh
                    p0 = hh * 64
                    sc = SC[pi % 2]
                    gq = min(qb // 4, 4)
                    Qh = Qt[p0:p0 + 64, lc, qb * 128:(qb + 1) * 128]
                    if qb >= 16:
                        kb0, kb1, nb = 0, -1, 0
                    else:
                        kb0, kb1 = max(qb - 1, 0), min(qb + 1, 15)
                        nb = (kb1 - kb0 + 1) * 128
                    lo = 512 - nb
                    nk = nb + 256
                    fns = []
                    if nb:
                        fns.append(MM(sc.ap[:, lo:512], Qh, Kt[p0:p0 + 64, kb0 * 128:(kb1 + 1) * 128], True, False))
                    fns.append(MM(sc.ap[:, 512:768], Qh, Kt[p0:p0 + 64, NLAT:T], True, True))
                    if nb:
                        mm_ = []
                        if kb0 == qb - 1:
                            mm_.append((lo, 0))
                        if kb1 == qb + 1:
                            mm_.append((512 - 128, 256))
                        for ii, (c_, mo) in enumerate(mm_):
                            fns.append(MM(sc.ap[:, c_:c_ + 128], IDB[:], MASK[:, mo:mo + 128], False, ii == len(mm_) - 1))
                    kregs = [Kr[4]] + ([Kr[kb // 4] for kb in range(kb0, kb1 + 1)] if nb else [])
                    S.op("pe", fns, R=[Qr[lc][gq], constR] + kregs, W=[sc.reg])
                    sm = SM.next()
                    skh = SK[:, l * 8 + head:l * 8 + head + 1]
                    S.op("dve", RED(sm.ap[:, 0:1], sc.ap[:, lo:768], ALU.max), R=[sc.reg], W=[sm.reg])
                    S.op("dve", TS(sm.ap[:, 1:2], sm.ap[:, 0:1], skh, -1.0, ALU.max, ALU.mult),
                         R=[sm.reg, constR], W=[sm.reg])
                    pf = PF[pi % 2]
                    S.op("act", ACTF(pf.ap[:, 0:nk], sc.ap[:, lo:768], AF.Exp,
                                     bias=sm.ap[:, 1:2], accum_out=sm.ap[:, 2:3]),
                         R=[sc.reg, sm.reg], W=[pf.reg, sm.reg])
                    sm2 = SM.next()
                    S.op("act", ACTF(sm2.ap[:, 0:1], skh, AF.Exp, bias=sm.ap[:, 1:2]), R=[sm.reg, constR], W=[sm2.reg])
                    st[pi] = (nk, nb, kb0, kb1, sm, sm2)

                def stage1b(pi):
                    nk, nb, kb0, kb1, sm, sm2 = st[pi]
                    pf = PF[pi % 2]
                    S.op("dve", TT(sm2.ap[:, 1:2], sm2.ap[:, 0:1], sm.ap[:, 2:3], ALU.add), R=[sm.reg, sm2.reg], W=[sm2.reg])
                    S.op("dve", RECIP(sm2.ap[:, 3:4], sm2.ap[:, 1:2]), R=[sm2.reg], W=[sm2.reg])
                    pn = PN[pi % 3]
                    S.op("dve", TS(pn.ap[:, 0:nk], pf.ap[:, 0:nk], sm2.ap[:, 3:4], None, ALU.mult),
                         R=[pf.reg, sm2.reg], W=[pn.reg])

                def stage2(pi):
                    nk, nb, kb0, kb1 = st[pi][:4]
                    pn, ptp, pt = PN[pi % 3], PTP[pi % 2], PTs[pi % 3]
                    S.op("pe", [TR(ptp.ap[:, jb * 128:(jb + 1) * 128], pn.ap[:, jb * 128:(jb + 1) * 128], IDB[:])
                                for jb in range(nk // 128)], R=[pn.reg, constR], W=[ptp.reg])
                    S.op("act", ACTF(pt.ap[:, 0:nk], ptp.ap[:, 0:nk], AF.Copy), R=[ptp.reg], W=[pt.reg])

                def stage3(pi):
                    lc, qb, hh = pairs[pi]
                    nk, nb, kb0, kb1 = st[pi][:4]
                    p0 = hh * 64
                    pt = PTs[pi % 3]
                    ob = OB[(pi // 2) % 2]
                    gq = min(qb // 4, 4)
                    tiles = (list(range(kb0, kb1 + 1)) if nb else []) + [16, 17]
                    S.op("pe", [MM(ob.ap[p0:p0 + 64, 0:128], Vt[:, tiles[jb], kvg * 64:(kvg + 1) * 64],
                                   pt.ap[:, jb * 128:(jb + 1) * 128], jb == 0, jb == len(tiles) - 1)
                                for jb in range(len(tiles))],
                         R=[pt.reg] + [Vr[i] for i in tiles], W=[ob.reg])
                    if hh == 1:
                        S.op("act", ACTF(Ot[:, lc, qb * 128:(qb + 1) * 128], ob.ap[:, 0:128], AF.Copy),
                             R=[ob.reg], W=[Or[lc][gq]])

                for step in range(len(pairs) + 3):
                    if step < len(pairs):
                        stage1(step)
                    if 0 <= step - 1 < len(pairs):
                        stage1b(step - 1)
                    if 0 <= step - 2 < len(pairs):
                        stage2(step - 2)
                    if 0 <= step - 3 < len(pairs):
                        stage3(step - 3)
                S.barrier()
                for m in range(KC):
                    for g in GO:
                        t0, n = TGS[g]
                        pb = MMB.next()
                        S.op("pe", [MM(pb.ap[:, :n], Xv[:, 2 * kvg + kk, m * 128:(m + 1) * 128],
                                       Ot[:, kk, t0:t0 + n], kk == 0, kk == 1) for kk in range(2)],
                             R=Xr + [Or[0][g], Or[1][g]], W=[pb.reg])
                        accum_H(l, 2, m, g, pb, n)
            S.barrier()

    def gates_compute(nt):
        bc = lambda ap: ap.unsqueeze(2).to_broadcast([128, nt, NEXP])
        gtb = TMP.next()
        gtR = gtb.reg
        GT = gtb.ap[:, 0:4 * NTILE * NEXP].rearrange("p (a t e) -> p a t e", a=4, t=NTILE)
        m1, m2, den = GT[:, 0, 0:nt, 0], GT[:, 0, 0:nt, 1], GT[:, 0, 0:nt, 2]
        e1, e2, e3 = GT[:, 1, 0:nt], GT[:, 2, 0:nt], GT[:, 3, 0:nt]
        LGv, GAv = LG[:, 0:nt], GATES[:, 0:nt]
        S.op("dve", RED(m1, LGv, ALU.max), R=[lgR], W=[gtR])
        S.op("dve", TT(e1, LGv, bc(m1), ALU.is_equal), R=[lgR, gtR], W=[gtR])
        S.op("dve", STT(e1, e1, -1e30, LGv, ALU.mult, ALU.add), R=[gtR, lgR], W=[gtR])
        S.op("dve", RED(m2, e1, ALU.max), R=[gtR], W=[gtR])
        S.op("dve", TT(e2, LGv, bc(m2), ALU.is_ge), R=[lgR, gtR], W=[gtR])
        S.op("dve", TT(e3, LGv, bc(m1), ALU.subtract), R=[lgR, gtR], W=[gtR])
        S.op("act", ACTF(e3, e3, AF.Exp), R=[gtR], W=[gtR])
        S.op("dve", TT(e2, e2, e3, ALU.mult), R=[gtR], W=[gtR])
        S.op("dve", RED(den, e2, ALU.add), R=[gtR], W=[gtR])
        S.op("dve", RECIP(den, den), R=[gtR], W=[gtR])
        S.op("dve", TT(GAv, e2, bc(den), ALU.mult), R=[gtR], W=[gatesR])

    BR3 = Ring([BB[1], BB[2], BB[3]])

    def ffn_prefetch(l):
        moe = (l % 2 == 1)
        i = l // 2
        w1s, w3s, w2s = (mw1[i, 0], mw3[i, 0], mw2[i, 0]) if moe else (dw1[i], dw3[i], dw2[i])
        blk = []
        for half in range(2):
            s = next_slot()
            si = (slot_i[0] - 1) % NSLOT
            col = (2 * half) * 128
            v1 = RINGT[:, si, 0:2048].rearrange("p (k n) -> p k n", k=KC)
            v3 = RINGT[:, si, 2048:4096].rearrange("p (k n) -> p k n", k=KC)
            v2 = RINGT[:, si, 4096:6144].rearrange("p (j n) -> p j n", j=2)
            S.dma("pool", v1, w1s[:, col:col + 256].rearrange("(k p) n -> p k n", p=128), W=[s[0].reg])
            S.dma("pool", v3, w3s[:, col:col + 256].rearrange("(k p) n -> p k n", p=128), W=[s[1].reg])
            S.dma("pool", v2, w2s[col:col + 256, :].rearrange("(j p) n -> p j n", p=128), W=[s[2].reg])
            blk.append((v1, v3, v2, s))
        return blk

    def ffn(l, pre, side=()):
        side = list(side)
        moe = (l % 2 == 1)
        i = l // 2
        nexp = NEXP if moe else 1
        nch = (DFF_MOE if moe else DFF_DENSE) // 128
        GO = range(4) if l == DEPTH - 1 else range(5)
        if moe:
            gates_compute(16 if l == DEPTH - 1 else NTILE)
        with ExitStack() as ph:
            Gt = sb("G", [128, 4, T], BF16, ph)
            Gr_ = [[Reg("g%d_%d" % (j, g)) for g in range(5)] for j in range(4)]
            if moe:
                GBC = sb("GBC", [128, T], F32, ph)
                gbcR = [Reg("gbc%d" % g) for g in range(5)]
            for e in range(nexp):
                if moe:
                    w1s, w3s, w2s = mw1[i, e], mw3[i, e], mw2[i, e]
                    for g in GO:
                        t0, n = TGS[g]
                        pb = BR3.next()
                        for ii in range(n // 128):
                            ti = t0 // 128 + ii
                            gb = TMP.next()
                            S.op("dve", COPY(gb.ap[:, 0:128], GATES[:, ti, e:e + 1].to_broadcast([128, 128])),
                                 R=[gatesR], W=[gb.reg])
                            S.op("pe", MM(pb.ap[:, ii * 128:(ii + 1) * 128], gb.ap[:, 0:128], IDF[:]),
                                 R=[gb.reg, constR], W=[pb.reg])
                        S.op("act", ACTF(GBC[:, t0:t0 + n], pb.ap[:, :n], AF.Copy), R=[pb.reg], W=[gbcR[g]])
                else:
                    w1s, w3s, w2s = dw1[i], dw3[i], dw2[i]
                for c0 in range(0, nch, 4):
                    nj = min(4, nch - c0)
                    blk = []
                    if e == 0 and c0 == 0:
                        blk = pre
                    for half in range((nj + 1) // 2 if not blk else 0):
                        s = next_slot()
                        si = (slot_i[0] - 1) % NSLOT
                        col = (c0 + 2 * half) * 128
                        v1 = RINGT[:, si, 0:2048].rearrange("p (k n) -> p k n", k=KC)
                        v3 = RINGT[:, si, 2048:4096].rearrange("p (k n) -> p k n", k=KC)
                        v2 = RINGT[:, si, 4096:6144].rearrange("p (j n) -> p j n", j=2)
                        S.dma("pool", v1, w1s[:, col:col + 256].rearrange("(k p) n -> p k n", p=128), W=[s[0].reg])
                        S.dma("pool", v3, w3s[:, col:col + 256].rearrange("(k p) n -> p k n", p=128), W=[s[1].reg])
                        S.dma("pool", v2, w2s[col:col + 256, :].rearrange("(j p) n -> p j n", p=128), W=[s[2].reg])
                        blk.append((v1, v3, v2, s))
                    for j in range(nj):
                        v1, v3, v2, s = blk[j // 2]
                        jc = (j % 2) * 128
                        for g in GO:
                            t0, n = TGS[g]
                            p1, p3 = MMB.next(), MMB.next()
                            S.op("pe", [MM(p1.ap[:, :n], v1[:, k, jc:jc + 128], A[:, k, t0:t0 + n],
                                           k == 0, k == KC - 1) for k in range(KC)],
                                 R=[s[0].reg] + [Ar[k][g] for k in range(KC)], W=[p1.reg])
                            S.op("pe", [MM(p3.ap[:, :n], v3[:, k, jc:jc + 128], A[:, k, t0:t0 + n],
                                           k == 0, k == KC - 1) for k in range(KC)],
                                 R=[s[1].reg] + [Ar[k][g] for k in range(KC)], W=[p3.reg])
                            sl = TMP.next()
                            S.op("act", ACTF(sl.ap[:, :n], p1.ap[:, :n], AF.Silu), R=[p1.reg], W=[sl.reg])
                            if moe:
                                tg_ = TMP.next()
                                S.op("dve", TT(tg_.ap[:, :n], p3.ap[:, :n], GBC[:, t0:t0 + n], ALU.mult),
                                     R=[p3.reg, gbcR[g]], W=[tg_.reg])
                                S.op("dve", TT(Gt[:, j, t0:t0 + n], tg_.ap[:, :n], sl.ap[:, :n], ALU.mult),
                                     R=[tg_.reg, sl.reg], W=[Gr_[j][g]])
                            else:
                                S.op("dve", TT(Gt[:, j, t0:t0 + n], p3.ap[:, :n], sl.ap[:, :n], ALU.mult),
                                     R=[p3.reg, sl.reg], W=[Gr_[j][g]])
                    for m in range(KC):
                        for g in GO:
                            t0, n = TGS[g]
                            pb = BR3.next()
                            S.op("pe", [MM(pb.ap[:, :n], blk[j // 2][2][:, j % 2, m * 128:(m + 1) * 128],
                                           Gt[:, j, t0:t0 + n], j == 0, j == nj - 1) for j in range(nj)],
                                 R=[b_[3][2].reg for b_ in blk] + [Gr_[j][g] for j in range(nj)], W=[pb.reg])
                            accum_H(l, 5, m, g, pb, n)
                    for _ in range(1 if moe else 2):
                        if side:
                            side.pop(0)()
            while side:
                side.pop(0)()
            S.barrier()

    def dump(src_is_A=False):
        for k in range(KC):
            S.dma("sp", dbg_d[k * 128:(k + 1) * 128, :], H[:, k, :], R=Hr[k], sem_reg=outR)

    outR = Reg("out")
    layers = list(layers)
    pre_tm = tm_prefetch(layers[0])
    run_ln_jobs(modulate_jobs(layers[0], 0, first=(layers[0] == 0)))
    for li, l in enumerate(layers):
        GO = range(4) if l == DEPTH - 1 else range(5)
        token_mixer(l, pre_tm)
        if dbg == ("mix", l):
            break
        pre_f = ffn_prefetch(l)
        run_ln_jobs(postnorm_jobs(l, 0, groups=GO)
                    + modulate_jobs(l, 1, router_i=(l // 2 if l % 2 == 1 else None), groups=GO))
        ffn(l, pre_f, side=([st_ for l2 in layers[li + 1:li + 3] for st_ in mods_steps(l2)] if (li == 1) else ()))
        if dbg == ("ffn", l):
            break
        nxt = []
        if li + 1 < len(layers):
            pre_tm = tm_prefetch(layers[li + 1])
            nxt = modulate_jobs(layers[li + 1], 0)
        run_ln_jobs(postnorm_jobs(l, 1, groups=GO) + nxt)
    if dbg is not None:
        dump()
    for k in range(KC):
        S.dma("sp", outT[k * 128:(k + 1) * 128, :], H[:, k, 0:NLAT], R=Hr[k][0:4], sem_reg=outR)
    S.eng["sp"].h.wait_ge(outR.dsem, outR.dcnt)
    es.close()
    return nc


def _host_consts():
    freqs = 10000.0 ** (-np.arange(16, dtype=np.float32) / 16)
    t = np.arange(NLAT)
    row = (t // 64).astype(np.float32)
    col = (t % 64).astype(np.float32)
    cos = np.zeros((128, NLAT), np.float32)
    sin = np.zeros((128, NLAT), np.float32)
    for p in range(128):
        d = p % 64
        blk = d // 16
        pos = row if blk < 2 else col
        ang = pos * freqs[d % 16]
        cos[p] = np.cos(ang)
        sin[p] = np.sin(ang) * (-1.0 if blk % 2 == 0 else 1.0)
    tabs = np.stack([cos.reshape(128, 4, 512), sin.reshape(128, 4, 512)], axis=2)
    qi = np.arange(128)[:, None]
    j = np.arange(128)[None, :]
    mask = np.zeros((128, 384), np.float32)
    mask[:, 0:128] = np.where(j >= qi, 0.0, MASKNEG)
    mask[:, 256:384] = np.where(j <= qi, 0.0, MASKNEG)
    edge = np.zeros((128, 4, 2, 8), np.float32)
    for gi, w in enumerate(POOL_WINDOWS):
        hw = w // 2
        for tt in range(hw):
            edge[:, gi, 0, tt] = 1.0 / (tt + hw)
        for ii in range(hw - 1):
            edge[:, gi, 1, ii] = 1.0 / (w - 1 - ii)
    return dict(
        tabs=np.ascontiguousarray(tabs),
        mask=mask.astype(ml_dtypes.bfloat16),
        identb=np.eye(128, dtype=np.float32).astype(ml_dtypes.bfloat16),
        identf=np.eye(128, dtype=np.float32),
        onesf=np.ones((128, 128), np.float32),
        onesd=np.full((128, 128), 1.0 / D, np.float32),
        epsc=np.tile(np.array([[LN_EPS, LN_EPS * ALPHA * ALPHA]], np.float32), (128, 1)),
        edge=edge.reshape(128, 64),
    )


def _win_ext_cols():
    def partner(cols):
        out = []
        for c in cols:
            d = c % 64
            out.append(c - d + (d + 16 if (d // 16) % 2 == 0 else d - 16))
        return out
    cols = list(range(768, 1280)) + list(range(640, 768))
    for kvg in range(2):
        q = list(range(kvg * 256, kvg * 256 + 256))
        kk = list(range(512 + kvg * 64, 512 + kvg * 64 + 64)) * 2
        cols += q + partner(q) + kk + partner(kk)
    assert len(cols) == WIN_EXT
    return np.array(cols)


def _prep_inputs(inp):
    f = lambda a: np.ascontiguousarray(np.asarray(a, dtype=np.float32))
    common = _host_consts()
    fm = lambda v: f(v).reshape(-1, 128).T
    common["badaT"] = np.ascontiguousarray(np.stack([fm(inp["b_ada"][l]) for l in range(DEPTH)], axis=1))
    lngb = np.zeros((128, DEPTH, 2, 2, KC), np.float32)
    for l in range(DEPTH):
        for s_ in range(2):
            lngb[:, l, s_, 0] = fm(inp["ln_g"][l, s_])
            lngb[:, l, s_, 1] = fm(inp["ln_b"][l, s_])
    common["lngb"] = lngb.reshape(128, -1)
    common["pscale"] = np.ascontiguousarray(np.stack([fm(inp["pool_scale"][l]) for l in range(DEPTH)], axis=1)).reshape(128, -1)
    common["sinkb"] = np.ascontiguousarray(np.broadcast_to(f(inp["sink"]).reshape(1, -1), (128, DEPTH * 8)))
    r = f(inp["router"]).reshape(2, KC, 128, NEXP).transpose(2, 0, 1, 3)
    common["routerT"] = np.ascontiguousarray(r).reshape(128, -1)
    common["w_ada"] = f(inp["w_ada"])
    common["w_inx"] = np.ascontiguousarray(f(inp["w_in"])[:, :, _win_ext_cols()])
    for k in ("w_pool", "w_out", "dense_w1", "dense_w3", "dense_w2", "moe_w1", "moe_w3", "moe_w2"):
        common[k] = f(inp[k])
    x, c, ctx, c_ctx = f(inp["x"]), f(inp["c"]), f(inp["ctx"]), f(inp["c_ctx"])
    maps = []
    for b in range(8):
        m = dict(common)
        m["xT"] = np.ascontiguousarray(x[b].T)
        m["ctxT"] = np.ascontiguousarray(ctx[b].T)
        m["cc"] = np.ascontiguousarray(np.stack([fm(c[b]), fm(c_ctx)], axis=2))
        maps.append(m)
    return maps


def kernel(**inputs):
    maps = _prep_inputs(inputs)
    nc = build()
    res = run_bass_kernel_spmd(nc, maps, core_ids=list(range(8)))
    out = np.stack([np.asarray(r["outT"]).T for r in res.results], axis=0)
    return np.ascontiguousarray(out.astype(np.float32))
```
